# Optimizing a Trainium2 kernel written in Bass

```python
import math
import jax, jax.numpy as jnp
from jax import lax
import numpy as np


D_MODEL = 2048
BATCH = 16
SEQ = 2048
DEPTH = 2

GRID_W = 64
CTX_LEN = 256
EPS = 1e-6
F32 = jnp.float32

NA_HEADS = 16
NA_HEAD_DIM = 64
NA_WIDTH = NA_HEADS * NA_HEAD_DIM
WIN_R = 8
WIN_C = 16
COL_BLOCK = 16
COL_BAND = 32

SSM_HEADS = 16
SSM_HEAD_DIM = 64
SSM_INNER = SSM_HEADS * SSM_HEAD_DIM
SSM_GROUPS = 2
SSM_STATE = 128
SSM_XBC = SSM_INNER + 2 * SSM_GROUPS * SSM_STATE
SSM_CONV = 5
SSM_CHUNK = 128

CONV_CH = 1024
CONV_K = 31

N_BRANCH = 3
IN_COLS = 3 * NA_WIDTH + SSM_INNER + SSM_XBC + 2 * SSM_HEADS + 2 * CONV_CH + N_BRANCH * D_MODEL

PEER_HEADS = 8
PEER_NKEYS = 128
PEER_KEY_DIM = 128
PEER_TOPK = 16
PEER_EXPERTS = PEER_NKEYS * PEER_NKEYS
PEER_CHUNK = 128

kernel_name = "hybrid_na_ssd_conformer_peer_dit"


def rmsnorm(x, g):
    xf = x.astype(F32)
    y = xf * lax.rsqrt(jnp.mean(xf * xf, axis=-1, keepdims=True) + EPS)
    return (y * g.astype(F32)).astype(x.dtype)


def layernorm(x, g, b):
    xf = x.astype(F32)
    mu = jnp.mean(xf, axis=-1, keepdims=True)
    xc = xf - mu
    y = xc * lax.rsqrt(jnp.mean(xc * xc, axis=-1, keepdims=True) + EPS)
    return (y * g.astype(F32) + b.astype(F32)).astype(x.dtype)


def modulate(x, g, shift, scale):
    return rmsnorm(x, g) * (1 + scale) + shift


def dwconv(x, w, b):
    k = w.shape[0]
    y = lax.conv_general_dilated(x, w[:, None, :].astype(x.dtype), window_strides=(1,),
                                 padding=[((k - 1) // 2, (k - 1) // 2)],
                                 dimension_numbers=('NWC', 'WIO', 'NWC'),
                                 feature_group_count=x.shape[-1])
    return y + b


def split_cols(p):
    sizes = (NA_WIDTH, NA_WIDTH, NA_WIDTH, SSM_INNER, SSM_XBC, 2 * SSM_HEADS, 2 * CONV_CH, N_BRANCH * D_MODEL)
    idx = np.cumsum(sizes)[:-1].tolist()
    return jnp.split(p, idx, axis=-1)


def to_heads(t):
    return t.reshape(t.shape[0], t.shape[1], NA_HEADS, NA_HEAD_DIM)


def na_static(rows):
    wr = min(WIN_R, rows)
    nb = GRID_W // COL_BLOCK
    j = np.arange(GRID_W)
    cstart = np.clip(j - WIN_C // 2, 0, GRID_W - WIN_C)
    bstart = np.clip(np.arange(nb) * COL_BLOCK - WIN_C // 2, 0, GRID_W - COL_BAND)
    band_cols = bstart[:, None] + np.arange(COL_BAND)[None, :]
    qcols = j.reshape(nb, COL_BLOCK)
    qcs = cstart.reshape(nb, COL_BLOCK)
    kcol = band_cols[:, None, :]
    mask = (kcol >= qcs[..., None]) & (kcol < qcs[..., None] + WIN_C)
    col_off = np.clip(kcol - qcols[..., None], -(WIN_C - 1), WIN_C - 1) + WIN_C - 1
    return wr, nb, band_cols, mask, col_off


def neighbourhood_attention(q, k, v, kc, vc, rpb):
    b, s, h, dh = q.shape
    rows = s // GRID_W
    wr, nb, band_cols, mask, col_off = na_static(rows)
    qg = (q * (dh ** -0.5)).reshape(b, rows, nb, COL_BLOCK, h, dh)
    kg = k.reshape(b, rows, GRID_W, h, dh)
    vg = v.reshape(b, rows, GRID_W, h, dh)
    col_bias = rpb.astype(F32)[:, :, col_off]
    win_mask = mask[:, :, None, :]

    def row_block(r):
        rs = jnp.clip(r - wr // 2, 0, rows - wr)
        kb = lax.dynamic_slice_in_dim(kg, rs, wr, axis=1)[:, :, band_cols]
        vb = lax.dynamic_slice_in_dim(vg, rs, wr, axis=1)[:, :, band_cols]
        qr = lax.dynamic_index_in_dim(qg, r, axis=1, keepdims=False)
        row_idx = rs + jnp.arange(wr) - r + WIN_R - 1
        bias = jnp.take(col_bias, row_idx, axis=1).transpose(0, 2, 3, 1, 4)
        s_win = jnp.einsum('bnihd,bwnkhd->bhniwk', qr, kb).astype(F32) + bias[None]
        s_win = jnp.where(win_mask, s_win, -jnp.inf).reshape(b, h, nb, COL_BLOCK, wr * COL_BAND)
        s_ctx = jnp.einsum('bnihd,bchd->bhnic', qr, kc).astype(F32)
        p = jax.nn.softmax(jnp.concatenate([s_win, s_ctx], axis=-1), axis=-1).astype(v.dtype)
        p_win = p[..., :wr * COL_BAND].reshape(b, h, nb, COL_BLOCK, wr, COL_BAND)
        p_ctx = p[..., wr * COL_BAND:]
        o = jnp.einsum('bhniwk,bwnkhd->bnihd', p_win, vb) + jnp.einsum('bhnic,bchd->bnihd', p_ctx, vc)
        return o.reshape(b, GRID_W, h, dh)

    out = lax.map(row_block, jnp.arange(rows))
    return out.transpose(1, 0, 2, 3, 4).reshape(b, s, h * dh)


def context_attention(qc, kc, vc):
    b, l, h, dh = qc.shape
    s = jnp.einsum('bqhd,bkhd->bhqk', qc * (dh ** -0.5), kc).astype(F32)
    p = jax.nn.softmax(s, axis=-1).astype(vc.dtype)
    return jnp.einsum('bhqk,bkhd->bqhd', p, vc).reshape(b, l, h * dh)


def segsum_exp(a):
    t = a.shape[-1]
    cs = jnp.cumsum(a, axis=-1)
    diff = cs[..., :, None] - cs[..., None, :]
    return jnp.exp(jnp.where(np.tril(np.ones((t, t), bool)), diff, -jnp.inf))


def ssd_scan(x, dt, A, bm, cm, h0, want_y):
    bsz, l, h, p = x.shape
    g, n = bm.shape[2], bm.shape[3]
    r = h // g
    q = SSM_CHUNK
    nc = l // q
    xc = (x.astype(F32) * dt[..., None]).reshape(bsz, nc, q, g, r, p)
    ac = (dt * A).reshape(bsz, nc, q, g, r).transpose(0, 1, 3, 4, 2)
    bc = bm.astype(F32).reshape(bsz, nc, q, g, n)
    cc = cm.astype(F32).reshape(bsz, nc, q, g, n)
    a_cs = jnp.cumsum(ac, axis=-1)
    decay_s = jnp.exp(a_cs[..., -1:] - a_cs).transpose(0, 1, 4, 2, 3)
    states = jnp.einsum('bclgn,bclgrp->bcgrpn', bc, xc * decay_s[..., None])
    states_all = jnp.concatenate([h0.reshape(bsz, 1, g, r, p, n), states], axis=1)
    chunk_a = jnp.pad(a_cs[..., -1], ((0, 0), (1, 0), (0, 0), (0, 0))).transpose(0, 2, 3, 1)
    new = jnp.einsum('bgrzc,bcgrpn->bzgrpn', segsum_exp(chunk_a), states_all)
    final = new[:, -1].reshape(bsz, h, p, n)
    if not want_y:
        return None, final
    prev = new[:, :-1]
    cb = jnp.einsum('bclgn,bcsgn->bcgls', cc, bc)
    m = cb[:, :, :, None] * segsum_exp(ac)
    y_diag = jnp.einsum('bcgrls,bcsgrp->bclgrp', m, xc)
    y_off = jnp.einsum('bclgn,bcgrpn->bclgrp', cc, prev) * jnp.exp(a_cs).transpose(0, 1, 4, 2, 3)[..., None]
    return (y_diag + y_off).reshape(bsz, l, h, p).astype(x.dtype), final


def bidirectional_ssd_mixer(zl, xbcl, dtl, zc, xbcc, dtc, conv_w, conv_b, dt_bias, A_log, D_skip,
                            norm_g, w_o, need_ctx):
    h, p, g, n = SSM_HEADS, SSM_HEAD_DIM, SSM_GROUPS, SSM_STATE

    def prep(xbc, dtr):
        xbc = jax.nn.silu(dwconv(xbc, conv_w, conv_b))
        bsz, l, _ = xbc.shape
        xs = xbc[..., :SSM_INNER].reshape(bsz, l, h, p)
        bm = xbc[..., SSM_INNER:SSM_INNER + g * n].reshape(bsz, l, g, n)
        cm = xbc[..., SSM_INNER + g * n:].reshape(bsz, l, g, n)
        dt = jax.nn.softplus(dtr.astype(F32).reshape(bsz, l, 2, h) + dt_bias.astype(F32))
        return xs, bm, cm, dt

    def gated_out(y, z):
        bsz, l = y.shape[:2]
        y = y.reshape(bsz, l, SSM_INNER) * jax.nn.silu(z)
        yg = y.reshape(bsz, l, g, SSM_INNER // g).astype(F32)
        yg = yg * lax.rsqrt(jnp.mean(yg * yg, axis=-1, keepdims=True) + EPS)
        return (yg.reshape(bsz, l, SSM_INNER) * norm_g.astype(F32)).astype(z.dtype) @ w_o

    xl, bl, cl, dtl_ = prep(xbcl, dtl)
    xc, bc, cc, dtc_ = prep(xbcc, dtc)
    h0 = jnp.zeros((xl.shape[0], h, p, n), F32)
    yl = D_skip[:, None] * xl
    yc = D_skip[:, None] * xc if need_ctx else None
    for d in range(2):
        A = -jnp.exp(A_log[d].astype(F32))
        f = (lambda t: t) if d == 0 else (lambda t: t[:, ::-1])
        yc_d, hc = ssd_scan(f(xc), f(dtc_[:, :, d]), A, f(bc), f(cc), h0, need_ctx)
        yl_d, _ = ssd_scan(f(xl), f(dtl_[:, :, d]), A, f(bl), f(cl), hc, True)
        yl = yl + f(yl_d)
        if need_ctx:
            yc = yc + f(yc_d)
    out_l = gated_out(yl, zl)
    out_c = gated_out(yc, zc) if need_ctx else None
    return out_l, out_c


def conformer_conv(u, dw_w, dw_b, ln_g, ln_b, w_o, b_o):
    a, gt = jnp.split(u, 2, axis=-1)
    hh = dwconv(a * jax.nn.sigmoid(gt), dw_w, dw_b)
    hh = layernorm(hh, ln_g, ln_b)
    return jax.nn.silu(hh) @ w_o + b_o


def merge_branches(gate_logits, y_att, y_ssm, y_cv, w_out):
    ga, gm, gc = jnp.split(jax.nn.sigmoid(gate_logits), 3, axis=-1)
    return (ga * y_att + gm * y_ssm + gc * y_cv) @ w_out


def peer(hh, wq, keys, u, v):
    t, d = hh.shape
    k = PEER_TOPK
    q = (hh @ wq).reshape(t, PEER_HEADS, 2, PEER_KEY_DIM)
    s = jnp.einsum('thjd,hjkd->thjk', q, keys).astype(F32)
    s1, i1 = lax.top_k(s[:, :, 0], k)
    s2, i2 = lax.top_k(s[:, :, 1], k)
    cand = (s1[..., :, None] + s2[..., None, :]).reshape(t, PEER_HEADS, k * k)
    best, pos = lax.top_k(cand, k)
    idx = jnp.take_along_axis(i1, pos // k, axis=-1) * PEER_NKEYS + jnp.take_along_axis(i2, pos % k, axis=-1)
    gate = jax.nn.softmax(best, axis=-1).astype(hh.dtype)
    nch = t // PEER_CHUNK
    hs = hh.reshape(nch, PEER_CHUNK, d)
    idxs = idx.reshape(nch, PEER_CHUNK, PEER_HEADS * k)
    gs = gate.reshape(nch, PEER_CHUNK, PEER_HEADS * k)

    def block(args):
        hb, ib, gb = args
        act = jax.nn.gelu(jnp.einsum('cd,ced->ce', hb, jnp.take(u, ib, axis=0)), approximate=False)
        return jnp.einsum('ce,ced->cd', gb * act, jnp.take(v, ib, axis=0))

    return lax.map(block, (hs, idxs, gs)).reshape(t, d)


def trunk_layer(xl, xc, c, c_ctx, need_ctx, w_mod, b_mod, norm1_g, norm2_g, w_in, na_rpb, na_wo,
                ssm_conv_w, ssm_conv_b, ssm_dt_bias, ssm_A_log, ssm_D, ssm_norm_g, ssm_wo,
                cv_dw_w, cv_dw_b, cv_ln_g, cv_ln_b, cv_wo, cv_bo, w_out, peer_wq, peer_keys, peer_u, peer_v):
    mod_l = jax.nn.silu(c) @ w_mod + b_mod
    mod_c = jax.nn.silu(c_ctx) @ w_mod + b_mod
    sh1, sc1, g1, sh2, sc2, g2 = jnp.split(mod_l[:, None, :], 6, axis=-1)
    csh1, csc1, cg1, csh2, csc2, cg2 = jnp.split(mod_c, 6, axis=-1)

    hl = modulate(xl, norm1_g, sh1, sc1)
    hc = modulate(xc, norm1_g, csh1, csc1)
    ql, kl, vl, zl, xbcl, dtl, glul, gatel = split_cols(hl @ w_in)
    qc, kc, vc, zc, xbcc, dtc, gluc, gatec = split_cols(hc @ w_in)
    kc_h, vc_h = to_heads(kc), to_heads(vc)
    y_att = neighbourhood_attention(to_heads(ql), to_heads(kl), to_heads(vl), kc_h, vc_h, na_rpb) @ na_wo
    y_ssm, y_ssm_c = bidirectional_ssd_mixer(zl, xbcl, dtl, zc, xbcc, dtc, ssm_conv_w, ssm_conv_b,
                                             ssm_dt_bias, ssm_A_log, ssm_D, ssm_norm_g, ssm_wo, need_ctx)
    y_cv = conformer_conv(glul, cv_dw_w, cv_dw_b, cv_ln_g, cv_ln_b, cv_wo, cv_bo)
    xl = xl + g1 * merge_branches(gatel, y_att, y_ssm, y_cv, w_out)
    if need_ctx:
        y_att_c = context_attention(to_heads(qc), kc_h, vc_h) @ na_wo
        y_cv_c = conformer_conv(gluc, cv_dw_w, cv_dw_b, cv_ln_g, cv_ln_b, cv_wo, cv_bo)
        xc = xc + cg1 * merge_branches(gatec, y_att_c, y_ssm_c, y_cv_c, w_out)

    d = xl.shape[-1]
    fl = modulate(xl, norm2_g, sh2, sc2).reshape(-1, d)
    if need_ctx:
        fc = modulate(xc, norm2_g, csh2, csc2).reshape(-1, d)
        out = peer(jnp.concatenate([fl, fc], axis=0), peer_wq, peer_keys, peer_u, peer_v)
        ol = out[:fl.shape[0]]
        xc = xc + cg2 * out[fl.shape[0]:].reshape(xc.shape)
    else:
        ol = peer(fl, peer_wq, peer_keys, peer_u, peer_v)
    xl = xl + g2 * ol.reshape(xl.shape)
    return xl, xc


def setup_inputs(seed: int = 0) -> dict:
    key = jax.random.key(seed)
    ks = jax.random.split(key, 32)
    L, D = DEPTH, D_MODEL

    def nrm(k, shape, s):
        return jax.random.normal(k, shape, F32) * s

    u01 = jax.random.uniform(ks[14], (L, 2, SSM_HEADS), F32)
    dt0 = jnp.exp(u01 * (math.log(0.1) - math.log(0.001)) + math.log(0.001))
    return {
        "x": nrm(ks[0], (BATCH, SEQ, D), 1.0),
        "c": nrm(ks[1], (BATCH, D), 1.0),
        "ctx": nrm(ks[2], (BATCH, CTX_LEN, D), 1.0),
        "c_ctx": nrm(ks[3], (D,), 1.0),
        "w_mod": nrm(ks[4], (L, D, 6 * D), 0.5 * D ** -0.5),
        "b_mod": nrm(ks[5], (L, 6 * D), 0.01),
        "norm1_g": 1.0 + nrm(ks[6], (L, D), 0.02),
        "norm2_g": 1.0 + nrm(ks[7], (L, D), 0.02),
        "w_in": nrm(ks[8], (L, D, IN_COLS), D ** -0.5),
        "na_rpb": nrm(ks[9], (L, NA_HEADS, 2 * WIN_R - 1, 2 * WIN_C - 1), 0.1),
        "na_wo": nrm(ks[10], (L, NA_WIDTH, D), NA_WIDTH ** -0.5),
        "ssm_conv_w": nrm(ks[11], (L, SSM_CONV, SSM_XBC), SSM_CONV ** -0.5),
        "ssm_conv_b": nrm(ks[12], (L, SSM_XBC), 0.01),
        "ssm_dt_bias": dt0 + jnp.log(-jnp.expm1(-dt0)),
        "ssm_A_log": jnp.log(jax.random.uniform(ks[13], (L, 2, SSM_HEADS), F32, minval=1.0, maxval=16.0)),
        "ssm_D": 1.0 + nrm(ks[15], (L, SSM_HEADS), 0.02),
        "ssm_norm_g": 1.0 + nrm(ks[16], (L, SSM_INNER), 0.02),
        "ssm_wo": nrm(ks[17], (L, SSM_INNER, D), SSM_INNER ** -0.5),
        "cv_dw_w": nrm(ks[18], (L, CONV_K, CONV_CH), CONV_K ** -0.5),
        "cv_dw_b": nrm(ks[19], (L, CONV_CH), 0.01),
        "cv_ln_g": 1.0 + nrm(ks[20], (L, CONV_CH), 0.02),
        "cv_ln_b": nrm(ks[21], (L, CONV_CH), 0.01),
        "cv_wo": nrm(ks[22], (L, CONV_CH, D), CONV_CH ** -0.5),
        "cv_bo": nrm(ks[23], (L, D), 0.01),
        "w_out": nrm(ks[24], (L, D, D), D ** -0.5),
        "peer_wq": nrm(ks[25], (L, D, PEER_HEADS * 2 * PEER_KEY_DIM), D ** -0.5),
        "peer_keys": nrm(ks[26], (L, PEER_HEADS, 2, PEER_NKEYS, PEER_KEY_DIM), PEER_KEY_DIM ** -0.5),
        "peer_u": nrm(ks[27], (L, PEER_EXPERTS, D), D ** -0.5),
        "peer_v": nrm(ks[28], (L, PEER_EXPERTS, D), 0.5),
        "final_norm_g": 1.0 + nrm(ks[29], (D,), 0.02),
    }


def reference(x, c, ctx, c_ctx, w_mod, b_mod, norm1_g, norm2_g, w_in, na_rpb, na_wo,
              ssm_conv_w, ssm_conv_b, ssm_dt_bias, ssm_A_log, ssm_D, ssm_norm_g, ssm_wo,
              cv_dw_w, cv_dw_b, cv_ln_g, cv_ln_b, cv_wo, cv_bo, w_out,
              peer_wq, peer_keys, peer_u, peer_v, final_norm_g):
    xl, xc = x, ctx
    for l in range(DEPTH):
        xl, xc = trunk_layer(xl, xc, c, c_ctx, l < DEPTH - 1, w_mod[l], b_mod[l], norm1_g[l], norm2_g[l],
                             w_in[l], na_rpb[l], na_wo[l], ssm_conv_w[l], ssm_conv_b[l], ssm_dt_bias[l],
                             ssm_A_log[l], ssm_D[l], ssm_norm_g[l], ssm_wo[l], cv_dw_w[l], cv_dw_b[l],
                             cv_ln_g[l], cv_ln_b[l], cv_wo[l], cv_bo[l], w_out[l],
                             peer_wq[l], peer_keys[l], peer_u[l], peer_v[l])
    return rmsnorm(xl, final_norm_g)
```

```python
import numpy as np
import ml_dtypes
import concourse.bass as bass
import concourse.mybir as mybir
from concourse.bass_utils import run_bass_kernel_spmd

F32 = mybir.dt.float32
BF16 = mybir.dt.bfloat16
I32 = mybir.dt.int32
U32 = mybir.dt.uint32
AF = mybir.ActivationFunctionType
ALU = mybir.AluOpType
AX = mybir.AxisListType

ENGS = ("tensor", "vector", "scalar", "gpsimd", "sync")
N_DMA_SEMS = 6

D = 2048
NT = 4608
NTILE = 36
INC = 13856
EPS = 1e-6
SEGS = [(0, 2048, 0), (2048, 2048, 1), (4096, 256, 2), (4352, 256, 2)]
NEG = -30000.0


class Tile:
    def __init__(self, t, name):
        self.t = t
        self.name = name
        self.st = {}

    def __getitem__(self, idx):
        return self.t[idx]


class Op:
    __slots__ = ("eng", "fn", "waits", "signal", "idx", "is_dma", "dma_slot", "dma_val", "sigval", "stage")

    def __init__(self, eng, fn, is_dma=False):
        self.eng = eng
        self.fn = fn
        self.waits = []
        self.signal = False
        self.is_dma = is_dma
        self.dma_slot = None
        self.dma_val = None
        self.sigval = None
        self.stage = None


class KB:
    def __init__(self, same_engine_sync=True):
        self.nc = bass.Bass("TRN2", target_bir_lowering=False)
        self.ops = {e: [] for e in ENGS}
        self.same_engine_sync = same_engine_sync
        self._ctx = []
        self.all_ops = []
        self.pending_dmas = []
        self.events = []

    def dram(self, name, shape, dtype, kind="Internal"):
        t = self.nc.dram_tensor(name, list(shape), dtype, kind=kind)
        return Tile(t.ap(), name)

    def sb(self, name, shape, dtype):
        self._uid = getattr(self, "_uid", 0) + 1
        name = "%s_%d" % (name, self._uid)
        g = self.nc.sbuf_tensor(name, list(shape), dtype)
        t = g.__enter__()
        self._ctx.append(g)
        return Tile(t, name)

    def ps(self, name, shape, dtype=F32):
        self._uid = getattr(self, "_uid", 0) + 1
        name = "%s_%d" % (name, self._uid)
        g = self.nc.psum_tensor(name, list(shape), dtype)
        t = g.__enter__()
        self._ctx.append(g)
        return Tile(t, name)

    def mark(self):
        return len(self._ctx)

    def release(self, mark):
        self.barrier()
        while len(self._ctx) > mark:
            g = self._ctx.pop()
            g.__exit__(None, None, None)

    @staticmethod
    def _norm(x):
        if isinstance(x, tuple):
            return x[0], x[1]
        return x, None

    def _deps(self, op, reads, writes):
        deps = []
        for x in reads:
            t, k = self._norm(x)
            for kk, st in t.st.items():
                if k is None or kk is None or kk == k:
                    if st[0] is not None:
                        deps.append(st[0])
        for x in writes:
            t, k = self._norm(x)
            for kk, st in t.st.items():
                if k is None or kk is None or kk == k:
                    if st[0] is not None:
                        deps.append(st[0])
                    deps.extend(st[1])
        for x in reads:
            t, k = self._norm(x)
            st = t.st.setdefault(k, [None, []])
            st[1].append(op)
        for x in writes:
            t, k = self._norm(x)
            if k is None:
                t.st = {None: [op, []]}
            else:
                t.st[k] = [op, []]
        return deps

    def op(self, eng, fn, reads=(), writes=(), is_dma=False, nobarrier=False):
        o = Op(eng, fn, is_dma)
        o.stage = getattr(self, "cur_stage", None)
        deps = self._deps(o, reads, writes)
        seen = set()
        for d in deps:
            if d is o or id(d) in seen:
                continue
            seen.add(id(d))
            if d.eng == eng and not d.is_dma and not is_dma:
                if not self.same_engine_sync:
                    continue
                if eng == "tensor":
                    continue
            o.waits.append(d)
        o.idx = len(self.ops[eng])
        self.ops[eng].append(o)
        self.all_ops.append(o)
        if is_dma and not nobarrier:
            self.pending_dmas.append(o)
        return o

    def barrier(self):
        lasts = []
        for e in ENGS:
            for o in reversed(self.ops[e]):
                if not o.is_dma and o.fn is not None:
                    lasts.append(o)
                    break
        dmas = list(self.pending_dmas)
        self.pending_dmas = []
        for e in ENGS:
            o = Op(e, None)
            o.waits = [d for d in lasts if d.eng != e] + dmas
            o.idx = len(self.ops[e])
            self.ops[e].append(o)
            self.all_ops.append(o)

    def dma(self, out, in_, reads=(), writes=(), q="sync", nobarrier=False, **kw):
        def fn(e, out=out, in_=in_, kw=kw):
            return e.dma_start(out=out, in_=in_, **kw)
        return self.op(q, fn, reads, writes, is_dma=True, nobarrier=nobarrier)

    def mm(self, out, lhsT, rhs, start, stop, reads=(), writes=(), **kw):
        def fn(e):
            return e.matmul(out, lhsT, rhs, start=start, stop=stop, **kw)
        return self.op("tensor", fn, reads, writes)

    def tr(self, out, in_, ident, reads=(), writes=()):
        def fn(e):
            return e.transpose(out, in_, ident)
        return self.op("tensor", fn, reads, writes)

    def v(self, method, *args, reads=(), writes=(), eng="vector", **kw):
        def fn(e):
            return getattr(e, method)(*args, **kw)
        return self.op(eng, fn, reads, writes)

    def act(self, out, in_, func, reads=(), writes=(), **kw):
        def fn(e):
            return e.activation(out=out, in_=in_, func=func, **kw)
        return self.op("scalar", fn, reads, writes)

    def build(self, final_waits=()):
        nc = self.nc
        nslots = {e: 0 for e in ENGS}
        for e in ENGS:
            j = 0
            for o in self.ops[e]:
                if o.is_dma:
                    o.dma_slot = j % N_DMA_SEMS
                    o.dma_val = 16 * (j // N_DMA_SEMS + 1)
                    j += 1
            nslots[e] = min(j, N_DMA_SEMS)
        for o in self.all_ops:
            for d in o.waits:
                d.signal = True
        for o in final_waits:
            o.signal = True
        for e in ENGS:
            c = 0
            for o in self.ops[e]:
                if o.signal and not o.is_dma:
                    c += 1
                    o.sigval = c
        sems = {}
        ctxs = []

        def mksem(name):
            g = nc.semaphore(name)
            s = g.__enter__()
            ctxs.append(g)
            return s
        for e in ENGS:
            sems[e] = mksem("s_" + e)
            for j in range(nslots[e]):
                sems[(e, j)] = mksem("d_%s_%d" % (e, j))

        def semval(d):
            if d.is_dma:
                return sems[(d.eng, d.dma_slot)], d.dma_val
            return sems[d.eng], d.sigval

        blk = nc.Block()
        block = blk.__enter__()

        def run_engine(ename, eng):
            known = {}
            cur = [None, None]

            def set_scope(name):
                if not getattr(self, "profile_scopes", False) or name == cur[0]:
                    return
                if cur[1] is not None:
                    cur[1].__exit__(None, None, None)
                    cur[1] = None
                cur[0] = name
                if name is not None:
                    cur[1] = nc.named_scope(name)
                    cur[1].__enter__()
            for o in self.ops[ename]:
                if o.fn is not None:
                    set_scope(o.stage)
                wl = {}
                for d in o.waits:
                    s, v = semval(d)
                    key = id(s)
                    if known.get(key, 0) >= v:
                        continue
                    if key not in wl or wl[key][1] < v:
                        wl[key] = (s, v)
                if o.is_dma:
                    s = sems[(ename, o.dma_slot)]
                    pv = o.dma_val - 16
                    if pv > 0 and known.get(id(s), 0) < pv:
                        if id(s) not in wl or wl[id(s)][1] < pv:
                            wl[id(s)] = (s, pv)
                for key, (s, v) in wl.items():
                    eng.wait_ge(s, v)
                    known[key] = v
                if o.fn is None:
                    continue
                ins = o.fn(eng)
                if o.is_dma:
                    ins.then_inc(sems[(ename, o.dma_slot)], 16)
                elif o.signal:
                    ins.then_inc(sems[ename], 1)
            set_scope(None)
            if ename == "sync":
                for d in final_waits:
                    s, v = semval(d)
                    eng.wait_ge(s, v)

        @block.sync
        def _(e):
            run_engine("sync", e)

        @block.tensor
        def _(e):
            run_engine("tensor", e)

        @block.vector
        def _(e):
            run_engine("vector", e)

        @block.scalar
        def _(e):
            run_engine("scalar", e)

        @block.gpsimd
        def _(e):
            run_engine("gpsimd", e)

        blk.__exit__(None, None, None)
        for g in reversed(ctxs):
            g.__exit__(None, None, None)
        while self._ctx:
            self._ctx.pop().__exit__(None, None, None)
        return nc


CST_IDENT, CST_TRIF, CST_TRIB, CST_STRF, CST_STRB, CST_ONES, CST_IOTA = range(7)


def host_consts():
    k = np.arange(128)[:, None]
    t = np.arange(128)[None, :]
    mats = [
        np.eye(128),
        (k <= t), (k >= t), (k > t), (k < t),
        np.ones((128, 128)),
        np.broadcast_to(t, (128, 128)),
    ]
    return np.concatenate([m.astype(np.float32) for m in mats], axis=1)


def host_bias_tables(rpb):
    L = rpb.shape[0]
    out = np.empty((L, 16, 5, 640, 128), np.float32)
    kl = np.arange(640)[:, None]
    ql = np.arange(128)[None, :]
    for ty, t in enumerate([0, 1, 2, 14, 15]):
        ks = int(np.clip(2 * t - 4, 0, 22))
        rk = ks + kl // 64
        jk = kl % 64
        rq = 2 * t + ql // 64
        jq = ql % 64
        rs = np.clip(rq - 4, 0, 24)
        cs = np.clip(jq - 8, 0, 48)
        valid = (rk >= rs) & (rk < rs + 8) & (jk >= cs) & (jk < cs + 16)
        ri = np.clip(rk - rq + 7, 0, 14)
        ci = np.clip(jk - jq + 15, 0, 30)
        g = rpb[:, :, ri, ci]
        out[:, :, ty] = np.where(valid[None, None], g, np.float32(NEG))
    return out


def tile_type(t):
    return {0: 0, 1: 1, 14: 3, 15: 4}.get(t, 2)


class Prog:
    def __init__(self, debug=(), nlayers=2, stages=None, profile=False):
        self.profile = profile
        self.kb = KB()
        self.debug = set(debug)
        self.nlayers = nlayers
        self.stages = stages
        self.finals = []

    def scratch(self, name, shape, dtype):
        kind = "ExternalOutput" if name in self.debug else "Internal"
        return self.kb.dram(name, shape, dtype, kind)

    def inp(self, name, shape, dtype=F32):
        return self.kb.dram(name, shape, dtype, "ExternalInput")

    def declare(self):
        L = 2
        s = self
        s.x_in = s.inp("x_c", [NT, D])
        s.c3T = s.inp("c3T", [D, 3])
        s.cst = s.inp("cst", [128, 7 * 128])
        s.w_mod = s.inp("w_mod", [L, D, 6 * D])
        s.b_mod = s.inp("b_mod", [L, 6 * D])
        s.norm1_g = s.inp("norm1_g", [L, D])
        s.norm2_g = s.inp("norm2_g", [L, D])
        s.w_in = s.inp("w_in", [L, D, INC])
        s.final_g = s.inp("final_norm_g", [1, D])
        s.biasT = s.inp("biasT", [L, 16, 5, 640, 128])
        s.AO = s.scratch("AO", [NT, 1024], BF16)
        s.ssm_cwT = s.inp("ssm_cwT", [L, 1536, 5])
        s.ssm_cb = s.inp("ssm_conv_b", [L, 1536])
        s.ssm_dtb = s.inp("ssm_dtb", [L, 32])
        s.ssm_Alog = s.inp("ssm_Alog", [L, 32])
        s.ssm_Dx = s.inp("ssm_Dx", [L, 1024])
        s.ssm_ng = s.inp("ssm_norm_g", [L, 1024])
        s.cv_wT = s.inp("cv_wT", [L, 1024, 31])
        s.cv_dw_b = s.inp("cv_dw_b", [L, 1024])
        s.cv_ln_g = s.inp("cv_ln_g", [L, 1024])
        s.cv_ln_b = s.inp("cv_ln_b", [L, 1024])
        s.cv_bo = s.inp("cv_bo", [L, 2048])
        s.na_wo = s.inp("na_wo", [L, 1024, D])
        s.ssm_wo = s.inp("ssm_wo", [L, 1024, D])
        s.cv_wo = s.inp("cv_wo", [L, 1024, D])
        s.w_out = s.inp("w_out", [L, D, D])
        s.NAW = [s.scratch("NAW%d" % l, [1024, D], BF16) for l in range(L)]
        s.SSW = [s.scratch("SSW%d" % l, [1024, D], BF16) for l in range(L)]
        s.CVW = [s.scratch("CVW%d" % l, [1024, D], BF16) for l in range(L)]
        s.WO = [s.scratch("WO%d" % l, [D, D], BF16) for l in range(L)]
        s.CVAT = s.scratch("CVAT", [1024, NT], BF16)
        s.peer_wq = s.inp("peer_wq", [L, D, D])
        s.keysT = s.inp("keysT", [L, 128, 16, 128])
        s.peer_uT = s.inp("peer_uT", [L, D, 16384])
        s.peer_v = s.inp("peer_v", [L, 16384, D])
        s.WQ = [s.scratch("WQ%d" % l, [D, D], BF16) for l in range(L)]
        s.UTB = [s.scratch("UTB%d" % l, [D, 16384], BF16) for l in range(L)]
        s.VB = [s.scratch("VB%d" % l, [16384, D], BF16) for l in range(L)]
        s.WDs = s.scratch("WDs", [128, 128, NT], BF16)
        s.SEL = s.scratch("SEL", [NT, 3, 128], F32)
        s.XBCc = s.scratch("XBCc", [512, NT], BF16)
        s.XTOK = s.scratch("XTOK", [NT, 1280], BF16)
        s.YD = [s.scratch("YD%d" % i, [NT, 1024], F32) for i in range(2)]
        s.YST = s.scratch("YST", [1024, NT], BF16)
        s.out = s.kb.dram("out", [4096, D], F32, "ExternalOutput")
        s.modd = s.scratch("modd", [3, 6 * D], F32)
        s.HT = s.scratch("HT", [D, NT], BF16)
        s.Wi = [s.scratch("Wi%d" % l, [D, INC], BF16) for l in range(L)]
        s.QT = s.scratch("QT", [1024, NT], BF16)
        s.KT = s.scratch("KT", [1024, NT], BF16)
        s.Vt = s.scratch("Vt", [NT, 1024], BF16)
        s.Zt = s.scratch("Zt", [NT, 1024], BF16)
        s.XBCT = s.scratch("XBCT", [1536, NT], BF16)
        s.DTt = s.scratch("DTt", [NT, 32], F32)
        s.GLUT = s.scratch("GLUT", [2048, NT], BF16)
        s.GATET = s.scratch("GATET", [6144, NT], BF16)
        s.xs = [s.scratch("xs%d" % i, [NT, D], F32) for i in range(2)]

    def load_consts(self):
        kb = self.kb
        self.cstsb = kb.sb("cstsb", [128, 7 * 128], F32)
        kb.dma(self.cstsb[:], self.cst[:], writes=[self.cstsb])
        self.identb = kb.sb("identb", [128, 128], BF16)
        kb.v("tensor_copy", self.identb[:], self.cstsb[:, 0:128], reads=[self.cstsb], writes=[self.identb])

    def C(self, i):
        return self.cstsb[:, i * 128:(i + 1) * 128]

    def cast_dram(self, dst, src_ap, rows, rows_per=512):
        kb = self.kb
        for i in range(0, rows, rows_per):
            r = min(rows_per, rows - i)
            kb.dma(dst[i:i + r, :], src_ap[i:i + r, :], writes=[(dst, ("c", i))], q="gpsimd", nobarrier=True)

    def prologue(self):
        self.cast_dram(self.Wi[0], self.w_in[0], D)

    def layer_casts(self, l):
        self.cast_dram(self.NAW[l], self.na_wo[l], 1024, 1024)
        self.cast_dram(self.SSW[l], self.ssm_wo[l], 1024, 1024)
        self.cast_dram(self.CVW[l], self.cv_wo[l], 1024, 1024)
        self.cast_dram(self.WO[l], self.w_out[l], D, 1024)
        self.cast_dram(self.WQ[l], self.peer_wq[l], D, 1024)
        self.cast_dram(self.UTB[l], self.peer_uT[l], D, 128)
        self.cast_dram(self.VB[l], self.peer_v[l], 16384, 1024)
        if l + 1 < self.nlayers:
            self.cast_dram(self.Wi[l + 1], self.w_in[l + 1], D)

    def stage_mod(self, l):
        kb = self.kb
        m0 = kb.mark()
        c3 = kb.sb("c3", [128, 16, 3], F32)
        kb.dma(c3[:], self.c3T[:].rearrange("(c p) r -> p c r", p=128), writes=[c3])
        c3s = kb.sb("c3s", [128, 16, 3], F32)
        kb.act(c3s[:], c3[:], AF.Silu, reads=[c3], writes=[c3s])
        wbuf = [kb.sb("wm%d" % i, [128, 16, 512], F32) for i in range(2)]
        bmb = [kb.sb("bm%d" % i, [3, 512], F32) for i in range(2)]
        pss = [kb.ps("pm%d" % i, [3, 512]) for i in range(2)]
        ob = [kb.sb("om%d" % i, [3, 512], F32) for i in range(2)]
        for nb in range(24):
            w = wbuf[nb % 2]
            bm = bmb[nb % 2]
            ps = pss[nb % 2]
            o = ob[nb % 2]
            kb.dma(w[:], self.w_mod[l, :, nb * 512:(nb + 1) * 512].rearrange("(c p) n -> p c n", p=128), writes=[w])
            kb.dma(bm[:], self.b_mod[l:l + 1, nb * 512:(nb + 1) * 512].broadcast_to([3, 512]), writes=[bm])
            for c in range(16):
                kb.mm(ps[:], c3s[:, c, :], w[:, c, :], c == 0, c == 15, reads=[c3s, w], writes=[ps])
            kb.v("tensor_tensor", o[:], ps[:], bm[:], ALU.add, reads=[ps, bm], writes=[o])
            kb.dma(self.modd[:, nb * 512:(nb + 1) * 512], o[:], reads=[o], writes=[(self.modd, nb)])
        kb.release(m0)

    def stage_norm(self, xin, gain_ap, shift_col, scale_col, HT, final_out=None):
        kb = self.kb
        m0 = kb.mark()
        gb = kb.sb("gb", [128, D], F32)
        kb.dma(gb[:], gain_ap.broadcast_to([128, D]), writes=[gb])
        A = []
        Bt = []
        if final_out is None:
            for r in range(3):
                a = kb.sb("nA%d" % r, [128, D], F32)
                b = kb.sb("nB%d" % r, [128, D], F32)
                kb.dma(a[:], self.modd[r:r + 1, scale_col * D:(scale_col + 1) * D].broadcast_to([128, D]),
                       reads=[self.modd], writes=[a])
                kb.dma(b[:], self.modd[r:r + 1, shift_col * D:(shift_col + 1) * D].broadcast_to([128, D]),
                       reads=[self.modd], writes=[b])
                kb.v("scalar_tensor_tensor", a[:], a[:], 1.0, gb[:], ALU.add, ALU.mult, reads=[a, gb], writes=[a])
                A.append(a)
                Bt.append(b)
        xt = [kb.sb("nx%d" % i, [128, D], F32) for i in range(2)]
        junk = kb.sb("njunk", [128, D], F32)
        ss = [kb.sb("nss%d" % i, [128, 1], F32) for i in range(2)]
        rs = [kb.sb("nrs%d" % i, [128, 1], F32) for i in range(2)]
        y32 = [kb.sb("ny32%d" % i, [128, D], F32) for i in range(2)]
        if final_out is None:
            yb = [kb.sb("nyb%d" % i, [128, D], BF16) for i in range(2)]
            pst = [kb.ps("npt%d" % i, [128, 16, 128], BF16) for i in range(2)]
            hT = [kb.sb("nhT%d" % i, [128, 16, 128], BF16) for i in range(2)]
        ntiles = NTILE if final_out is None else 32
        for t in range(ntiles):
            i = t % 2
            r = 0 if t < 16 else (1 if t < 32 else 2)
            kb.dma(xt[i][:], xin[t * 128:(t + 1) * 128, :], reads=[(xin, t)], writes=[xt[i]])
            kb.act(junk[:], xt[i][:], AF.Square, reads=[xt[i]], writes=[junk, ss[i]], accum_out=ss[i][:])
            kb.act(rs[i][:], ss[i][:], AF.Sqrt, reads=[ss[i]], writes=[rs[i]], scale=1.0 / D, bias=self.epsb[:])
            kb.v("reciprocal", rs[i][:], rs[i][:], reads=[rs[i]], writes=[rs[i]])
            if final_out is not None:
                kb.v("scalar_tensor_tensor", y32[i][:], xt[i][:], rs[i][:], gb[:], ALU.mult, ALU.mult,
                     reads=[xt[i], rs[i], gb], writes=[y32[i]])
                o = kb.dma(final_out[t * 128:(t + 1) * 128, :], y32[i][:], reads=[y32[i]], writes=[(final_out, t)])
                self.finals.append(o)
                continue
            kb.v("scalar_tensor_tensor", y32[i][:], xt[i][:], rs[i][:], A[r][:], ALU.mult, ALU.mult,
                 reads=[xt[i], rs[i], A[r]], writes=[y32[i]])
            kb.v("tensor_tensor", yb[i][:], y32[i][:], Bt[r][:], ALU.add, reads=[y32[i], Bt[r]], writes=[yb[i]], eng="gpsimd")
            for c in range(16):
                kb.tr(pst[i][:, c, :], yb[i][:, c * 128:(c + 1) * 128], self.identb[:],
                      reads=[yb[i], self.identb], writes=[(pst[i], c)])
            kb.act(hT[i][:], pst[i][:], AF.Copy, reads=[pst[i]], writes=[hT[i]])
            kb.dma(HT[:, t * 128:(t + 1) * 128].rearrange("(c p) t -> p c t", p=128), hT[i][:],
                   reads=[hT[i]], writes=[(HT, t)])
        kb.release(m0)

    def stage_proj(self, l):
        kb = self.kb
        m0 = kb.mark()
        Wi = self.Wi[l]
        secs = [(self.QT, "f", 0, 1024), (self.KT, "f", 1024, 1024), (self.Vt, "t", 2048, 1024),
                (self.Zt, "t", 3072, 1024), (self.XBCT, "f", 4096, 1536), (self.DTt, "t", 5632, 32),
                (self.GLUT, "f", 5664, 2048), (self.GATET, "f", 7712, 6144)]
        blocks = []
        for dst, kind, c0, n in secs:
            for j in range(0, n, 512):
                blocks.append((dst, kind, c0 + j, j, min(512, n - j)))
        TG = 1536
        hbuf = [kb.sb("ph%d" % i, [128, 16, TG], BF16) for i in range(2)]
        wbuf = [kb.sb("pw%d" % i, [128, 16, 512], BF16) for i in range(3)]
        pss = [kb.ps("pp%d" % i, [128, 512]) for i in range(4)]
        obf = [kb.sb("pob%d" % i, [128, 512], BF16) for i in range(4)]
        o32 = [kb.sb("po32%d" % i, [128, 32], F32) for i in range(2)]
        NG = NT // TG
        items = [(tg, blk) for tg in range(NG) for blk in blocks]

        def load_h(tg):
            kb.dma(hbuf[tg % 2][:], self.HT[:, tg * TG:(tg + 1) * TG].rearrange("(c p) t -> p c t", p=128),
                   reads=[self.HT], writes=[hbuf[tg % 2]])

        def load_w(i):
            tg, (dst, kind, wc0, dc0, n) = items[i]
            kb.dma(wbuf[i % 3][:, :, 0:n], Wi[:, wc0:wc0 + n].rearrange("(c p) n -> p c n", p=128), reads=[Wi],
                   writes=[wbuf[i % 3]])
        load_h(0)
        load_w(0)
        load_w(1)
        cnt = 0
        for i, (tg, (dst, kind, wc0, dc0, n)) in enumerate(items):
            if i + 2 < len(items):
                load_w(i + 2)
            if i % len(blocks) == 0 and tg + 1 < NG:
                load_h(tg + 1)
            h = hbuf[tg % 2]
            w = wbuf[i % 3]
            t0 = tg * TG
            if kind == "f":
                for sub in range(n // 128):
                    for tb in range(TG // 512):
                        ps = pss[cnt % 4]
                        ob = obf[cnt % 4]
                        cnt += 1
                        for c in range(16):
                            kb.mm(ps[:], w[:, c, sub * 128:(sub + 1) * 128], h[:, c, tb * 512:(tb + 1) * 512],
                                  c == 0, c == 15, reads=[w, h], writes=[ps])
                        kb.act(ob[:], ps[:], AF.Copy, reads=[ps], writes=[ob])
                        r0 = dc0 + sub * 128
                        c0 = t0 + tb * 512
                        kb.dma(dst[r0:r0 + 128, c0:c0 + 512], ob[:], reads=[ob], writes=[(dst, ("p", c0, r0))], q="scalar")
            else:
                for tt in range(TG // 128):
                    ps = pss[cnt % 4]
                    ob = obf[cnt % 4]
                    cnt += 1
                    for c in range(16):
                        kb.mm(ps[:, 0:n], h[:, c, tt * 128:(tt + 1) * 128], w[:, c, 0:n], c == 0, c == 15,
                              reads=[w, h], writes=[ps])
                    rows = slice(t0 + tt * 128, t0 + (tt + 1) * 128)
                    if n == 32:
                        o = o32[tt % 2]
                        kb.act(o[:], ps[:, 0:32], AF.Copy, reads=[ps], writes=[o])
                        kb.dma(dst[rows, :], o[:], reads=[o], writes=[(dst, ("p", tg, tt))], q="scalar")
                    else:
                        kb.act(ob[:], ps[:], AF.Copy, reads=[ps], writes=[ob])
                        kb.dma(dst[rows, dc0:dc0 + 512], ob[:], reads=[ob], writes=[(dst, ("p", tg, tt, dc0))], q="scalar")
        kb.release(m0)

    def stage_attn(self, l):
        kb = self.kb
        m0 = kb.mark()
        bias = [kb.sb("abias%d" % i, [128, 25, 128], F32) for i in range(2)]
        qts = [kb.sb("aq%d" % i, [64, 2304], BF16) for i in range(2)]
        kts = [kb.sb("ak%d" % i, [64, 2304], BF16) for i in range(2)]
        vas = [kb.sb("av%d" % i, [128, 18, 65], BF16) for i in range(2)]
        aos = [kb.sb("ao%d" % i, [128, 18, 64], BF16) for i in range(2)]
        tmps = [kb.sb("atmp%d" % i, [128, 640], F32) for i in range(2)]
        pTs = [kb.sb("apT%d" % i, [128, 896], BF16) for i in range(2)]
        rds = [kb.sb("ard%d" % i, [128, 1], F32) for i in range(2)]
        pss = [kb.ps("aps%d" % i, [128, 2, 512]) for i in range(2)]
        pso = [kb.ps("apo%d" % i, [128, 65]) for i in range(2)]
        for va in vas:
            kb.v("memset", va[:], 1.0, writes=[va])
        it = 0
        hb = 0
        for h in range(16):
            bt = bias[h % 2]
            kb.dma(bt[:], self.biasT[l, h].rearrange("ty (c k) q -> k (ty c) q", k=128), writes=[bt])
            for b in range(2):
                qt, kt, va, ao = qts[hb % 2], kts[hb % 2], vas[hb % 2], aos[hb % 2]
                hb += 1
                l0, c0t = b * 2048, 4096 + b * 256
                hs = slice(h * 64, (h + 1) * 64)
                kb.dma(qt[:, 0:2048], self.QT[hs, l0:l0 + 2048], reads=[self.QT], writes=[(qt, 0)])
                kb.dma(qt[:, 2048:2304], self.QT[hs, c0t:c0t + 256], reads=[self.QT], writes=[(qt, 1)])
                kb.dma(kt[:, 0:2048], self.KT[hs, l0:l0 + 2048], reads=[self.KT], writes=[(kt, 0)])
                kb.dma(kt[:, 2048:2304], self.KT[hs, c0t:c0t + 256], reads=[self.KT], writes=[(kt, 1)])
                kb.dma(va[:, 0:16, 0:64], self.Vt[l0:l0 + 2048, hs].rearrange("(c p) d -> p c d", p=128),
                       reads=[self.Vt], writes=[(va, 0)])
                kb.dma(va[:, 16:18, 0:64], self.Vt[c0t:c0t + 256, hs].rearrange("(c p) d -> p c d", p=128),
                       reads=[self.Vt], writes=[(va, 1)])
                for t in range(18):
                    i = it % 2
                    it += 1
                    ps, po, tmp, pT, rd = pss[i], pso[i], tmps[i], pTs[i], rds[i]
                    if t < 16:
                        cc0 = int(np.clip(2 * t - 4, 0, 22)) // 2
                        chunks = [cc0 + j for j in range(5)] + [16, 17]
                        ty = tile_type(t)
                    else:
                        chunks = [16, 17]
                    for j, ch in enumerate(chunks):
                        kb.mm(ps[:, j // 4, (j % 4) * 128:(j % 4 + 1) * 128], kt[:, ch * 128:(ch + 1) * 128],
                              qt[:, t * 128:(t + 1) * 128], True, True, reads=[kt, qt], writes=[ps])
                    if t < 16:
                        kb.v("scalar_tensor_tensor", tmp[:, 0:512], ps[:, 0, :], 0.125,
                             bt[:, ty * 5:ty * 5 + 4, :], ALU.mult, ALU.add, reads=[ps, bt], writes=[tmp])
                        kb.v("scalar_tensor_tensor", tmp[:, 512:640], ps[:, 1, 0:128], 0.125,
                             bt[:, ty * 5 + 4, :], ALU.mult, ALU.add, reads=[ps, bt], writes=[tmp])
                        kb.act(pT[:, 0:640], tmp[:], AF.Exp, reads=[tmp], writes=[pT])
                        kb.act(pT[:, 640:896], ps[:, 1, 128:384], AF.Exp, reads=[ps], writes=[pT], scale=0.125)
                    else:
                        kb.act(pT[:, 0:256], ps[:, 0, 0:256], AF.Exp, reads=[ps], writes=[pT], scale=0.125)
                    n = len(chunks)
                    for j, ch in enumerate(chunks):
                        kb.mm(po[:], pT[:, j * 128:(j + 1) * 128], va[:, ch, :], j == 0, j == n - 1,
                              reads=[pT, va], writes=[po])
                    kb.v("reciprocal", rd[:], po[:, 64:65], reads=[po], writes=[rd])
                    kb.v("tensor_scalar", ao[:, t, :], po[:, 0:64], rd[:], None, ALU.mult, reads=[po, rd], writes=[ao])
                kb.dma(self.AO[l0:l0 + 2048, hs].rearrange("(c p) d -> p c d", p=128), ao[:, 0:16, :],
                       reads=[ao], writes=[(self.AO, ("l", h, b))])
                kb.dma(self.AO[c0t:c0t + 256, hs].rearrange("(c p) d -> p c d", p=128), ao[:, 16:18, :],
                       reads=[ao], writes=[(self.AO, ("c", h, b))])
        kb.release(m0)

    def build_diags(self, name, wT_ap, nch, K):
        kb = self.kb
        wc = kb.sb(name + "_wc", [128, nch, K], F32)
        kb.dma(wc[:], wT_ap.rearrange("(c p) k -> p c k", p=128), writes=[wc])
        dgs = []
        for c in range(nch):
            dg = kb.sb("%s_dg%d" % (name, c), [128, K, 128], BF16)
            for k in range(K):
                kb.v("tensor_scalar", dg[:, k, :], self.C(CST_IDENT), wc[:, c, k:k + 1], None, ALU.mult,
                     reads=[wc, self.cstsb], writes=[(dg, k)], eng=("vector" if (c + k) % 2 == 0 else "gpsimd"))
            dgs.append(dg)
        return dgs

    def stage_ssm_conv(self, l):
        kb = self.kb
        m0 = kb.mark()
        dgs = self.build_diags("sc", self.ssm_cwT[l], 12, 5)
        cb = kb.sb("sc_cb", [128, 12], F32)
        kb.dma(cb[:], self.ssm_cb[l].rearrange("(c p) -> p c", p=128), writes=[cb], allow_slow_non_contiguous=True)
        xin = [kb.sb("sc_x%d" % i, [128, 12, 516], BF16) for i in range(2)]
        cvb = [kb.sb("sc_cv%d" % i, [128, 512], BF16) for i in range(3)]
        pss = [kb.ps("sc_ps%d" % i, [128, 512]) for i in range(2)]
        pst = [kb.ps("sc_pt%d" % i, [128, 4, 128], BF16) for i in range(2)]
        tok = [kb.sb("sc_tok%d" % i, [128, 4, 1280], BF16) for i in range(2)]
        blk = 0
        cnt = 0
        for (s0, SL, _r) in SEGS:
            N = min(512, SL)
            for t0 in range(0, SL, N):
                x = xin[blk % 2]
                tk = tok[blk % 2]
                blk += 1
                lo = max(t0 - 2, 0)
                hi = min(t0 + N + 2, SL)
                kb.v("memset", x[:], 0.0, writes=[x], eng="gpsimd")
                kb.dma(x[:, :, lo - (t0 - 2):hi - (t0 - 2)],
                       self.XBCT[:, s0 + lo:s0 + hi].rearrange("(c p) t -> p c t", p=128), reads=[self.XBCT], writes=[x])
                for c in range(12):
                    ps = pss[cnt % 2]
                    cv = cvb[cnt % 3]
                    pt = pst[cnt % 2]
                    cnt += 1
                    for k in range(5):
                        kb.mm(ps[:, 0:N], dgs[c][:, k, :], x[:, c, k:k + N], k == 0, k == 4, reads=[dgs[c], x], writes=[ps])
                    kb.act(cv[:, 0:N], ps[:, 0:N], AF.Silu, reads=[ps, cb], writes=[cv], bias=cb[:, c:c + 1])
                    if c >= 8:
                        kb.dma(self.XBCc[(c - 8) * 128:(c - 7) * 128, s0 + t0:s0 + t0 + N], cv[:, 0:N], reads=[cv],
                               writes=[(self.XBCc, (c, s0 + t0))])
                    if c < 10:
                        for tt in range(N // 128):
                            kb.tr(pt[:, tt, :], cv[:, tt * 128:(tt + 1) * 128], self.identb[:], reads=[cv, self.identb],
                                  writes=[(pt, tt)])
                        kb.v("tensor_copy", tk[:, 0:N // 128, c * 128:(c + 1) * 128], pt[:, 0:N // 128, :], reads=[pt],
                             writes=[(tk, c)])
                kb.dma(self.XTOK[s0 + t0:s0 + t0 + N, :].rearrange("(t p) c -> p t c", p=128), tk[:, 0:N // 128, :],
                       reads=[tk], writes=[(self.XTOK, s0 + t0)])
        kb.release(m0)

    def stage_ssm_scan(self, l):
        kb = self.kb
        m0 = kb.mark()
        dtb = kb.sb("ss_dtb", [128, 32], F32)
        kb.dma(dtb[:], self.ssm_dtb[l:l + 1, :].broadcast_to([128, 32]), writes=[dtb])
        nA = kb.sb("ss_nA", [128, 32], F32)
        kb.dma(nA[:], self.ssm_Alog[l:l + 1, :].broadcast_to([128, 32]), writes=[nA])
        kb.act(nA[:], nA[:], AF.Exp, reads=[nA], writes=[nA])
        kb.v("tensor_scalar", nA[:], nA[:], -1.0, None, ALU.mult, reads=[nA], writes=[nA])
        dt = kb.sb("ss_dt", [128, NTILE, 32], F32)
        aa = kb.sb("ss_a", [128, NTILE, 32], F32)
        t1 = kb.sb("ss_t1", [128, NTILE, 32], F32)
        t2 = kb.sb("ss_t2", [128, NTILE, 32], F32)
        kb.dma(dt[:], self.DTt[:, :].rearrange("(t p) c -> p t c", p=128), reads=[self.DTt], writes=[dt])
        bc = lambda tl: tl[:].unsqueeze(1).broadcast_to([128, NTILE, 32])
        kb.v("tensor_tensor", dt[:], dt[:], bc(dtb), ALU.add, reads=[dt, dtb], writes=[dt])
        kb.v("tensor_scalar", t1[:], dt[:], -1.0, None, ALU.mult, reads=[dt], writes=[t1])
        kb.v("tensor_tensor", t1[:], t1[:], dt[:], ALU.max, reads=[t1, dt], writes=[t1])
        kb.act(t1[:], t1[:], AF.Exp, reads=[t1], writes=[t1], scale=-1.0)
        kb.v("tensor_scalar", t1[:], t1[:], 1.0, None, ALU.add, reads=[t1], writes=[t1])
        kb.act(t2[:], t1[:], AF.Ln, reads=[t1], writes=[t2])
        kb.v("tensor_scalar", dt[:], dt[:], 0.0, None, ALU.max, reads=[dt], writes=[dt])
        kb.v("tensor_tensor", dt[:], dt[:], t2[:], ALU.add, reads=[dt, t2], writes=[dt])
        kb.v("tensor_tensor", aa[:], dt[:], bc(nA), ALU.mult, reads=[dt, nA], writes=[aa])

        NS = 4
        xtk = [[kb.sb("ss_x%d_%d" % (s_, i), [128, 1280], BF16) for i in range(2)] for s_ in range(NS)]
        bct = [[kb.sb("ss_bc%d_%d" % (s_, i), [128, 4, 128], BF16) for i in range(2)] for s_ in range(NS)]
        cs_sb = [kb.sb("ss_cs%d" % i, [128, 16], F32) for i in range(NS)]
        dif = [kb.sb("ss_dif%d" % i, [128, 16], F32) for i in range(NS)]
        dIn = [kb.sb("ss_din%d" % i, [128, 16], F32) for i in range(NS)]
        dOut = [kb.sb("ss_dout%d" % i, [128, 16], F32) for i in range(NS)]
        dAs = [kb.sb("ss_dA%d" % i, [128, 16], F32) for i in range(NS)]
        xdt = [kb.sb("ss_xdt%d" % i, [128, 16, 64], BF16) for i in range(NS)]
        xdec = [kb.sb("ss_xdec%d" % i, [128, 16, 64], BF16) for i in range(NS)]
        GTm = [kb.sb("ss_gtm%d" % i, [128, 128], F32) for i in range(4)]
        A1 = [kb.sb("ss_a1%d" % i, [128, 128], F32) for i in range(4)]
        LT = [kb.sb("ss_lt%d" % i, [128, 128], F32) for i in range(4)]
        MT = [kb.sb("ss_mt%d" % i, [128, 128], BF16) for i in range(4)]
        yo = [kb.sb("ss_yo%d" % i, [128, 512], F32) for i in range(4)]
        ych = [kb.sb("ss_ych%d" % i, [128, 1024], F32) for i in range(NS)]
        st32 = [kb.sb("ss_st32%d" % i, [128, 2, 512], F32) for i in range(NS)]
        stbf = [kb.sb("ss_stbf%d" % i, [128, 2, 512], BF16) for i in range(NS)]
        ps_sm = kb.ps("ss_psm0", [128, 32])
        ps_g = kb.ps("ss_pg", [128, 128])
        ps_d = [kb.ps("ss_pd%d" % i, [128, 128]) for i in range(2)]
        ps_y = kb.ps("ss_py", [128, 2, 512])
        ps_off = kb.ps("ss_poff", [128, 512])
        ps_st = kb.ps("ss_pst", [128, 512])
        cnts = {"h": 0, "g": 0}

        def stream(si, b, d):
            tri = self.C(CST_TRIF if d == 0 else CST_TRIB)
            strict = self.C(CST_STRF if d == 0 else CST_STRB)
            kb.v("memset", st32[si][:], 0.0, writes=[st32[si]])
            kb.v("memset", stbf[si][:], 0.0, writes=[stbf[si]])
            ctx_t = [32 + 2 * b, 33 + 2 * b]
            lat_t = [16 * b + c for c in range(16)]
            order = (ctx_t + lat_t) if d == 0 else (ctx_t[::-1] + lat_t[::-1])

            def load(k):
                tt = order[k]
                kb.dma(xtk[si][k % 2][:], self.XTOK[tt * 128:(tt + 1) * 128, :], reads=[self.XTOK], writes=[xtk[si][k % 2]])
                kb.dma(bct[si][k % 2][:], self.XBCc[:, tt * 128:(tt + 1) * 128].rearrange("(c p) t -> p c t", p=128),
                       reads=[self.XBCc], writes=[bct[si][k % 2]])
            load(0)
            for k, tt in enumerate(order):
                if k + 1 < len(order):
                    load(k + 1)
                i = si
                x, bt = xtk[si][k % 2], bct[si][k % 2]
                a_c = aa[:, tt, d * 16:(d + 1) * 16]
                dt_c = dt[:, tt, d * 16:(d + 1) * 16]
                psm = ps_sm
                kb.mm(psm[:, 0:16], tri, a_c, True, True, reads=[aa, self.cstsb], writes=[psm])
                kb.mm(psm[:, 16:32], self.C(CST_ONES), a_c, True, True, reads=[aa, self.cstsb], writes=[psm])
                kb.v("tensor_copy", cs_sb[i][:], psm[:, 0:16], reads=[psm], writes=[cs_sb[i]])
                kb.v("tensor_tensor", dif[i][:], psm[:, 16:32], cs_sb[i][:], ALU.subtract, reads=[psm, cs_sb[i]],
                     writes=[dif[i]])
                kb.act(dAs[i][:], psm[:, 16:32], AF.Exp, reads=[psm], writes=[dAs[i]])
                kb.act(dIn[i][:], cs_sb[i][:], AF.Exp, reads=[cs_sb[i]], writes=[dIn[i]])
                kb.act(dOut[i][:], dif[i][:], AF.Exp, reads=[dif[i]], writes=[dOut[i]])
                xv = x[:, 0:1024].rearrange("p (h e) -> p h e", e=64)
                kb.v("tensor_tensor", xdt[i][:], xv, dt_c.unsqueeze(2).broadcast_to([128, 16, 64]), ALU.mult,
                     reads=[x, dt], writes=[xdt[i]], eng="gpsimd")
                kb.v("tensor_tensor", xdec[i][:], xdt[i][:], dOut[i][:].unsqueeze(2).broadcast_to([128, 16, 64]),
                     ALU.mult, reads=[xdt[i], dOut[i]], writes=[xdec[i]], eng="gpsimd")
                for g in range(2):
                    gm = GTm[cnts["g"] % 4]
                    yg = yo[cnts["g"] % 4]
                    cnts["g"] += 1
                    kb.mm(ps_g[:], bt[:, g, :], bt[:, 2 + g, :], True, True, reads=[bt], writes=[ps_g])
                    kb.v("tensor_tensor", gm[:], ps_g[:], tri, ALU.mult, reads=[ps_g, self.cstsb], writes=[gm])
                    for hh in range(8):
                        h = g * 8 + hh
                        j = cnts["h"] % 4
                        pd = ps_d[cnts["h"] % 2]
                        cnts["h"] += 1
                        kb.v("tensor_scalar", A1[j][:], strict, a_c[:, h:h + 1], None, ALU.mult,
                             reads=[aa, self.cstsb], writes=[A1[j]])
                        kb.mm(pd[:], A1[j][:], tri, True, True, reads=[A1[j], self.cstsb], writes=[pd])
                        kb.act(LT[j][:], pd[:], AF.Exp, reads=[pd], writes=[LT[j]])
                        kb.v("tensor_tensor", MT[j][:], LT[j][:], gm[:], ALU.mult, reads=[LT[j], gm], writes=[MT[j]],
                             eng="gpsimd")
                        kb.mm(ps_y[:, g, hh * 64:(hh + 1) * 64], MT[j][:], xdt[i][:, h, :], True, True,
                              reads=[MT[j], xdt[i]], writes=[(ps_y, g)])
                    kb.mm(ps_off[:], bt[:, 2 + g, :], stbf[si][:, g, :], True, True, reads=[bt, (stbf[si], g)], writes=[ps_off])
                    kb.v("tensor_tensor", yg[:].rearrange("p (h e) -> p h e", e=64),
                         ps_off[:].rearrange("p (h e) -> p h e", e=64),
                         dIn[i][:, g * 8:(g + 1) * 8].unsqueeze(2).broadcast_to([128, 8, 64]), ALU.mult,
                         reads=[ps_off, dIn[i]], writes=[yg])
                    kb.v("tensor_tensor", ych[i][:, g * 512:(g + 1) * 512], ps_y[:, g, :], yg[:], ALU.add,
                         reads=[(ps_y, g), yg], writes=[(ych[i], g)])
                    kb.mm(ps_st[:], x[:, 1024 + g * 128:1024 + (g + 1) * 128],
                          xdec[i][:, g * 8:(g + 1) * 8, :].rearrange("p h e -> p (h e)"), True, True,
                          reads=[x, xdec[i]], writes=[ps_st])
                    kb.v("tensor_tensor", st32[si][:, g, :].rearrange("p (h e) -> p h e", e=64),
                         st32[si][:, g, :].rearrange("p (h e) -> p h e", e=64),
                         dAs[i][:, g * 8:(g + 1) * 8].unsqueeze(2).broadcast_to([128, 8, 64]), ALU.mult,
                         reads=[(st32[si], g), dAs[i]], writes=[(st32[si], g)])
                    kb.v("tensor_tensor", st32[si][:, g, :], st32[si][:, g, :], ps_st[:], ALU.add,
                         reads=[(st32[si], g), ps_st], writes=[(st32[si], g)])
                    kb.act(stbf[si][:, g, :], st32[si][:, g, :], AF.Copy, reads=[(st32[si], g)], writes=[(stbf[si], g)])
                kb.dma(self.YD[d][tt * 128:(tt + 1) * 128, :], ych[i][:], reads=[ych[i]], writes=[(self.YD[d], tt)])
                yield

        gens = [stream(si, si // 2, si % 2) for si in range(NS)]
        alive = list(gens)
        while alive:
            for g_ in list(alive):
                try:
                    next(g_)
                except StopIteration:
                    alive.remove(g_)
        kb.release(m0)

    def stage_ssm_out(self, l):
        kb = self.kb
        m0 = kb.mark()
        Dx = kb.sb("so_D", [128, 1024], F32)
        kb.dma(Dx[:], self.ssm_Dx[l:l + 1, :].broadcast_to([128, 1024]), writes=[Dx])
        ng = kb.sb("so_ng", [128, 1024], F32)
        kb.dma(ng[:], self.ssm_ng[l:l + 1, :].broadcast_to([128, 1024]), writes=[ng])
        yf = [kb.sb("so_yf%d" % i, [128, 1024], F32) for i in range(2)]
        yb = [kb.sb("so_yb%d" % i, [128, 1024], F32) for i in range(2)]
        xx = [kb.sb("so_x%d" % i, [128, 1024], BF16) for i in range(2)]
        zz = [kb.sb("so_z%d" % i, [128, 1024], BF16) for i in range(2)]
        sz = [kb.sb("so_sz%d" % i, [128, 1024], F32) for i in range(2)]
        junk = kb.sb("so_junk", [128, 512], F32)
        ss = [kb.sb("so_ss%d" % i, [128, 2], F32) for i in range(2)]
        yn = [kb.sb("so_yn%d" % i, [128, 1024], BF16) for i in range(2)]
        pt = [kb.ps("so_pt%d" % i, [128, 8, 128], BF16) for i in range(2)]
        yT = [kb.sb("so_yT%d" % i, [128, 8, 128], BF16) for i in range(2)]
        for tt in range(NTILE):
            i = tt % 2
            rows = slice(tt * 128, (tt + 1) * 128)
            kb.dma(yf[i][:], self.YD[0][rows, :], reads=[self.YD[0]], writes=[yf[i]])
            kb.dma(yb[i][:], self.YD[1][rows, :], reads=[self.YD[1]], writes=[yb[i]])
            kb.dma(xx[i][:], self.XTOK[rows, 0:1024], reads=[self.XTOK], writes=[xx[i]])
            kb.dma(zz[i][:], self.Zt[rows, :], reads=[self.Zt], writes=[zz[i]])
            kb.v("tensor_tensor", yf[i][:], yf[i][:], yb[i][:], ALU.add, reads=[yf[i], yb[i]], writes=[yf[i]])
            kb.v("tensor_tensor", yb[i][:], xx[i][:], Dx[:], ALU.mult, reads=[xx[i], Dx], writes=[yb[i]], eng="gpsimd")
            kb.v("tensor_tensor", yf[i][:], yf[i][:], yb[i][:], ALU.add, reads=[yf[i], yb[i]], writes=[yf[i]])
            kb.act(sz[i][:], zz[i][:], AF.Silu, reads=[zz[i]], writes=[sz[i]])
            kb.v("tensor_tensor", yf[i][:], yf[i][:], sz[i][:], ALU.mult, reads=[yf[i], sz[i]], writes=[yf[i]])
            for g in range(2):
                kb.act(junk[:], yf[i][:, g * 512:(g + 1) * 512], AF.Square, reads=[yf[i]], writes=[junk, (ss[i], g)],
                       accum_out=ss[i][:, g:g + 1])
            kb.act(ss[i][:], ss[i][:], AF.Sqrt, reads=[ss[i]], writes=[ss[i]], scale=1.0 / 512, bias=self.epsb[:])
            kb.v("reciprocal", ss[i][:], ss[i][:], reads=[ss[i]], writes=[ss[i]])
            for g in range(2):
                kb.v("scalar_tensor_tensor", yn[i][:, g * 512:(g + 1) * 512], yf[i][:, g * 512:(g + 1) * 512],
                     ss[i][:, g:g + 1], ng[:, g * 512:(g + 1) * 512], ALU.mult, ALU.mult, reads=[yf[i], ss[i], ng],
                     writes=[(yn[i], g)])
            for c in range(8):
                kb.tr(pt[i][:, c, :], yn[i][:, c * 128:(c + 1) * 128], self.identb[:], reads=[yn[i], self.identb],
                      writes=[(pt[i], c)])
            kb.act(yT[i][:], pt[i][:], AF.Copy, reads=[pt[i]], writes=[yT[i]])
            kb.dma(self.YST[:, rows].rearrange("(c p) t -> p c t", p=128), yT[i][:], reads=[yT[i]],
                   writes=[(self.YST, tt)])
        kb.release(m0)

    def stage_conf(self, l):
        kb = self.kb
        m0 = kb.mark()
        dgs = self.build_diags("cf", self.cv_wT[l], 8, 31)
        small = kb.sb("cf_small", [128, 3, 8], F32)
        for j, src in enumerate([self.cv_dw_b, self.cv_ln_g, self.cv_ln_b]):
            kb.dma(small[:, j, :], src[l].rearrange("(c p) -> p c", p=128), writes=[(small, j)], allow_slow_non_contiguous=True)
        ga = [kb.sb("cf_ga%d" % i, [128, 16, 542], BF16) for i in range(2)]
        sig = kb.sb("cf_sig", [128, 8, 542], BF16)
        u = kb.sb("cf_u", [128, 8, 542], BF16)
        hh = kb.sb("cf_hh", [128, 8, 512], F32)
        sq = kb.sb("cf_sq", [128, 8, 512], F32)
        mean = kb.sb("cf_mean", [128, 512], F32)
        m2 = kb.sb("cf_m2", [128, 512], F32)
        rstd = kb.sb("cf_rstd", [128, 512], F32)
        tt1 = [kb.sb("cf_t1%d" % i, [128, 512], F32) for i in range(2)]
        ob = [kb.sb("cf_ob%d" % i, [128, 512], BF16) for i in range(2)]
        pss = [kb.ps("cf_ps%d" % i, [128, 512]) for i in range(2)]
        ps1 = kb.ps("cf_sum1", [128, 512])
        ps2 = kb.ps("cf_sum2", [128, 512])
        blk = 0
        for (s0, SL, _r) in SEGS:
            N = min(512, SL)
            for t0 in range(0, SL, N):
                g = ga[blk % 2]
                blk += 1
                lo = max(t0 - 15, 0)
                hi = min(t0 + N + 15, SL)
                W = N + 30
                kb.v("memset", g[:], 0.0, writes=[g], eng="gpsimd")
                kb.dma(g[:, :, lo - (t0 - 15):hi - (t0 - 15)],
                       self.GLUT[:, s0 + lo:s0 + hi].rearrange("(c p) t -> p c t", p=128), reads=[self.GLUT], writes=[g])
                kb.act(sig[:, :, 0:W], g[:, 8:16, 0:W], AF.Sigmoid, reads=[g], writes=[sig])
                kb.v("tensor_tensor", u[:, :, 0:W], g[:, 0:8, 0:W], sig[:, :, 0:W], ALU.mult, reads=[g, sig], writes=[u])
                for c in range(8):
                    ps = pss[c % 2]
                    for k in range(31):
                        kb.mm(ps[:, 0:N], dgs[c][:, k, :], u[:, c, k:k + N], k == 0, k == 30, reads=[dgs[c], u], writes=[ps])
                    kb.act(hh[:, c, 0:N], ps[:, 0:N], AF.Identity, reads=[ps, small], writes=[(hh, c)], bias=small[:, 0, c:c + 1])
                    kb.act(sq[:, c, 0:N], hh[:, c, 0:N], AF.Square, reads=[(hh, c)], writes=[(sq, c)])
                for c in range(8):
                    kb.mm(ps1[:, 0:N], self.C(CST_ONES), hh[:, c, 0:N], c == 0, c == 7, reads=[(hh, c), self.cstsb], writes=[ps1])
                for c in range(8):
                    kb.mm(ps2[:, 0:N], self.C(CST_ONES), sq[:, c, 0:N], c == 0, c == 7, reads=[(sq, c), self.cstsb], writes=[ps2])
                kb.v("tensor_scalar", mean[:, 0:N], ps1[:, 0:N], 1.0 / 1024, None, ALU.mult, reads=[ps1], writes=[mean])
                kb.v("tensor_tensor", m2[:, 0:N], mean[:, 0:N], mean[:, 0:N], ALU.mult, reads=[mean], writes=[m2])
                kb.v("scalar_tensor_tensor", rstd[:, 0:N], ps2[:, 0:N], 1.0 / 1024, m2[:, 0:N], ALU.mult, ALU.subtract,
                     reads=[ps2, m2], writes=[rstd])
                kb.act(rstd[:, 0:N], rstd[:, 0:N], AF.Sqrt, reads=[rstd], writes=[rstd], bias=self.epsb[:])
                kb.v("reciprocal", rstd[:, 0:N], rstd[:, 0:N], reads=[rstd], writes=[rstd])
                for c in range(8):
                    t1 = tt1[c % 2]
                    o = ob[c % 2]
                    kb.v("tensor_tensor", t1[:, 0:N], hh[:, c, 0:N], mean[:, 0:N], ALU.subtract, reads=[(hh, c), mean],
                         writes=[t1], eng="gpsimd")
                    kb.v("tensor_tensor", t1[:, 0:N], t1[:, 0:N], rstd[:, 0:N], ALU.mult, reads=[t1, rstd], writes=[t1])
                    kb.act(o[:, 0:N], t1[:, 0:N], AF.Silu, reads=[t1, small], writes=[o], scale=small[:, 1, c:c + 1],
                           bias=small[:, 2, c:c + 1])
                    kb.dma(self.CVAT[c * 128:(c + 1) * 128, s0 + t0:s0 + t0 + N], o[:, 0:N], reads=[o],
                           writes=[(self.CVAT, (c, s0 + t0))])
        kb.release(m0)

    def stage_merge(self, l, xin, xout):
        kb = self.kb
        m0 = kb.mark()
        g1 = []
        for r in range(3):
            t = kb.sb("mg_g1%d" % r, [128, D], F32)
            kb.dma(t[:], self.modd[r:r + 1, 2 * D:3 * D].broadcast_to([128, D]), reads=[self.modd], writes=[t])
            g1.append(t)
        bo = kb.sb("mg_bo", [128, 16], F32)
        kb.dma(bo[:], self.cv_bo[l].rearrange("(c p) -> p c", p=128), writes=[bo], allow_slow_non_contiguous=True)
        aot = [kb.sb("mg_ao%d" % i, [128, 1024], BF16) for i in range(2)]
        AOT = kb.sb("mg_AOT", [128, 8, 512], BF16)
        YS = kb.sb("mg_YS", [128, 8, 512], BF16)
        CV = kb.sb("mg_CV", [128, 8, 512], BF16)
        wts = [kb.sb("mg_w%d" % i, [128, 3, 8, 128], BF16) for i in range(2)]
        gts = [kb.sb("mg_gt%d" % i, [128, 3, 512], BF16) for i in range(2)]
        sg = [kb.sb("mg_sg%d" % i, [128, 3, 512], F32) for i in range(2)]
        ma = [kb.sb("mg_ma%d" % i, [128, 512], F32) for i in range(2)]
        mb = [kb.sb("mg_mb%d" % i, [128, 512], F32) for i in range(2)]
        mc = [kb.sb("mg_mc%d" % i, [128, 512], F32) for i in range(2)]
        MT = kb.sb("mg_MT", [128, 16, 512], BF16)
        wo = [kb.sb("mg_wo%d" % i, [128, 16, 512], BF16) for i in range(2)]
        xt = kb.sb("mg_xt", [128, 4, D], F32)
        tmp = [kb.sb("mg_tmp%d" % i, [128, 512], F32) for i in range(2)]
        pt = [kb.ps("mg_pt%d" % i, [128, 8, 128], BF16) for i in range(2)]
        psa = kb.ps("mg_psa", [128, 512])
        pss_ = kb.ps("mg_pss", [128, 512])
        psc = kb.ps("mg_psc", [128, 512])
        pso = [kb.ps("mg_pso%d" % i, [128, 512]) for i in range(2)]
        cnt = 0
        for tg in range(9):
            t0 = tg * 512
            r = 0 if tg < 4 else (1 if tg < 8 else 2)
            for tt in range(4):
                a = aot[tt % 2]
                p = pt[tt % 2]
                kb.dma(a[:], self.AO[t0 + tt * 128:t0 + (tt + 1) * 128, :], reads=[self.AO], writes=[a])
                for c in range(8):
                    kb.tr(p[:, c, :], a[:, c * 128:(c + 1) * 128], self.identb[:], reads=[a, self.identb], writes=[(p, c)])
                kb.act(AOT[:, :, tt * 128:(tt + 1) * 128], p[:], AF.Copy, reads=[p], writes=[(AOT, tt)])
            kb.dma(YS[:], self.YST[:, t0:t0 + 512].rearrange("(c p) t -> p c t", p=128), reads=[self.YST], writes=[YS])
            kb.dma(CV[:], self.CVAT[:, t0:t0 + 512].rearrange("(c p) t -> p c t", p=128), reads=[self.CVAT], writes=[CV])
            kb.dma(xt[:], xin[t0:t0 + 512, :].rearrange("(t p) d -> p t d", p=128), reads=[xin], writes=[xt])
            for fc in range(16):
                i = fc % 2
                w = wts[i]
                for j, src in enumerate([self.NAW[l], self.SSW[l], self.CVW[l]]):
                    kb.dma(w[:, j, :, :], src[:, fc * 128:(fc + 1) * 128].rearrange("(c p) f -> p c f", p=128),
                           reads=[src], writes=[(w, j)])
                kb.dma(gts[i][:], self.GATET[:, t0:t0 + 512].rearrange("(j c p) t -> p j c t", j=3, p=128)[:, :, fc, :],
                       reads=[self.GATET], writes=[gts[i]])
                kb.act(sg[i][:], gts[i][:], AF.Sigmoid, reads=[gts[i]], writes=[sg[i]])
                for j, (ps, src) in enumerate([(psa, AOT), (pss_, YS), (psc, CV)]):
                    for c in range(8):
                        kb.mm(ps[:], w[:, j, c, :], src[:, c, :], c == 0, c == 7, reads=[w, src], writes=[ps])
                kb.v("tensor_tensor", ma[i][:], psa[:], sg[i][:, 0, :], ALU.mult, reads=[psa, sg[i]], writes=[ma[i]])
                kb.v("tensor_tensor", mb[i][:], pss_[:], sg[i][:, 1, :], ALU.mult, reads=[pss_, sg[i]], writes=[mb[i]])
                kb.v("scalar_tensor_tensor", mc[i][:], psc[:], bo[:, fc:fc + 1], sg[i][:, 2, :], ALU.add, ALU.mult,
                     reads=[psc, bo, sg[i]], writes=[mc[i]])
                kb.v("tensor_tensor", ma[i][:], ma[i][:], mb[i][:], ALU.add, reads=[ma[i], mb[i]], writes=[ma[i]], eng="gpsimd")
                kb.v("tensor_tensor", MT[:, fc, :], ma[i][:], mc[i][:], ALU.add, reads=[ma[i], mc[i]], writes=[(MT, fc)],
                     eng="gpsimd")
            for nb in range(4):
                wb = wo[nb % 2]
                kb.dma(wb[:], self.WO[l][:, nb * 512:(nb + 1) * 512].rearrange("(c p) n -> p c n", p=128),
                       reads=[self.WO[l]], writes=[wb])
                for tt in range(4):
                    po = pso[cnt % 2]
                    tm = tmp[cnt % 2]
                    cnt += 1
                    for fc in range(16):
                        kb.mm(po[:], MT[:, fc, tt * 128:(tt + 1) * 128], wb[:, fc, :], fc == 0, fc == 15,
                              reads=[MT, wb], writes=[po])
                    cs = slice(nb * 512, (nb + 1) * 512)
                    kb.v("tensor_tensor", tm[:], po[:], g1[r][:, cs], ALU.mult, reads=[po, g1[r]], writes=[tm])
                    kb.v("tensor_tensor", xt[:, tt, cs], xt[:, tt, cs], tm[:], ALU.add, reads=[(xt, (tt, nb)), tm],
                         writes=[(xt, (tt, nb))], eng="gpsimd")
            kb.dma(xout[t0:t0 + 512, :].rearrange("(t p) d -> p t d", p=128), xt[:], reads=[xt], writes=[(xout, tg)])
        kb.release(m0)

    def top16_multi(self, items):
        kb = self.kb
        for (va, wa, vo, io, rd, wr) in items:
            kb.v("max", vo[:, 0:8], va, reads=rd, writes=wr)
        for (va, wa, vo, io, rd, wr) in items:
            kb.v("max_index", io[:, 0:8], vo[:, 0:8], va, reads=rd + wr, writes=wr)
        for (va, wa, vo, io, rd, wr) in items:
            kb.v("match_replace", wa, vo[:, 0:8], va, -1e30, reads=rd + wr, writes=wr)
        for (va, wa, vo, io, rd, wr) in items:
            kb.v("max", vo[:, 8:16], wa, reads=wr, writes=wr)
        for (va, wa, vo, io, rd, wr) in items:
            kb.v("max_index", io[:, 8:16], vo[:, 8:16], wa, reads=wr, writes=wr)

    def stage_peer_route(self, l):
        kb = self.kb
        m0 = kb.mark()
        kT = kb.sb("pr_kT", [128, 16, 128], F32)
        kb.dma(kT[:], self.keysT[l], writes=[kT])
        io16 = self.C(CST_IOTA)[:, 0:16]
        hb = [kb.sb("pr_h%d" % i, [128, 16, 512], BF16) for i in range(1)]
        wq = [kb.sb("pr_wq%d" % i, [128, 16, 128], BF16) for i in range(2)]
        qT = kb.sb("pr_qT", [128, 16, 512], F32)
        sc = kb.sb("pr_sc", [128, 16, 128], F32)
        scw = kb.sb("pr_scw", [128, 16, 128], F32)
        stop = kb.sb("pr_stop", [128, 16, 16], F32)
        itop = kb.sb("pr_itop", [128, 16, 16], U32)
        itf = kb.sb("pr_itf", [128, 16, 16], F32)
        cand = kb.sb("pr_cand", [128, 8, 256], F32)
        candw = kb.sb("pr_candw", [128, 8, 256], F32)
        best = kb.sb("pr_best", [128, 8, 16], F32)
        pos = kb.sb("pr_pos", [128, 8, 16], U32)
        pu = kb.sb("pr_pu", [128, 2, 8, 16], U32)
        pf = kb.sb("pr_pf", [128, 2, 8, 16], F32)
        eq = [kb.sb("pr_eq0", [128, 8, 16, 16], F32)] * 2
        sel = kb.sb("pr_sel", [128, 3, 128], F32)
        ex = kb.sb("pr_ex", [128, 8, 16], F32)
        sm = kb.sb("pr_sm", [128, 8], F32)
        selT = [kb.sb("pr_selT%d" % i, [128, 3, 128], F32) for i in range(2)]
        TQ = 16
        Abig = [kb.sb("pr_A%d" % i, [128, TQ, 128], BF16) for i in range(2)]
        Bbig = [kb.sb("pr_B%d" % i, [128, TQ, 128], BF16) for i in range(2)]
        WDsb = [kb.sb("pr_WD0", [128, 128, 256], BF16)] * 2
        ps_q = kb.ps("pr_psq", [128, 512])
        ps_s = kb.ps("pr_pss", [128, 16, 128])
        ps_t = kb.ps("pr_pst", [128, 3, 128])
        ps_w = [kb.ps("pr_psw%d" % i, [128, 4, 128]) for i in range(2)]
        iota3 = self.C(CST_IOTA).unsqueeze(1).broadcast_to([128, TQ, 128])
        cn = {'ab': 0, 'wi': 0}
        def front(tg, tt):
            if True:
                tile_i = tg * 4 + tt
                sT = selT[tile_i % 2]
                for fc in range(16):
                    kb.mm(ps_s[:, fc, :], qT[:, fc, tt * 128:(tt + 1) * 128], kT[:, fc, :], True, True,
                          reads=[(qT, fc), kT], writes=[ps_s])
                kb.act(sc[:], ps_s[:], AF.Copy, reads=[ps_s], writes=[sc])
                self.top16_multi([(sc[:, fc, :], scw[:, fc, :], stop[:, fc, :], itop[:, fc, :], [sc],
                                   [(scw, fc), (stop, fc), (itop, fc)]) for fc in range(16)])
                kb.v("tensor_copy", itf[:], itop[:], reads=[itop], writes=[itf])
                sv = stop[:].rearrange("p (h j) i -> p h j i", j=2)
                iv = itf[:].rearrange("p (h j) i -> p h j i", j=2)
                kb.v("tensor_tensor", cand[:].rearrange("p h (i j) -> p h i j", j=16),
                     sv[:, :, 0, :].unsqueeze(3).broadcast_to([128, 8, 16, 16]),
                     sv[:, :, 1, :].unsqueeze(2).broadcast_to([128, 8, 16, 16]), ALU.add, reads=[stop], writes=[cand])
                self.top16_multi([(cand[:, hh, :], candw[:, hh, :], best[:, hh, :], pos[:, hh, :], [cand],
                                   [(candw, hh), (best, hh), (pos, hh)]) for hh in range(8)])
                kb.v("tensor_single_scalar", pu[:, 0], pos[:], 4, ALU.logical_shift_right, reads=[pos], writes=[(pu, 0)])
                kb.v("tensor_single_scalar", pu[:, 1], pos[:], 15, ALU.bitwise_and, reads=[pos], writes=[(pu, 1)])
                kb.v("tensor_copy", pf[:], pu[:], reads=[pu], writes=[pf])
                for j in range(2):
                    e = eq[j]
                    kb.v("tensor_tensor", e[:], io16.unsqueeze(1).unsqueeze(1).broadcast_to([128, 8, 16, 16]),
                         pf[:, j].unsqueeze(3).broadcast_to([128, 8, 16, 16]), ALU.is_equal, reads=[pf, self.cstsb], writes=[e])
                    kb.v("tensor_tensor", e[:], e[:], iv[:, :, j, :].unsqueeze(2).broadcast_to([128, 8, 16, 16]), ALU.mult,
                         reads=[e, itf], writes=[e])
                    kb.v("tensor_reduce", sel[:, j, :].rearrange("p (h m) -> p h m", m=16), e[:], AX.X, ALU.add,
                         reads=[e], writes=[(sel, j)])
                kb.v("tensor_tensor", ex[:], best[:], best[:, :, 0:1].broadcast_to([128, 8, 16]), ALU.subtract,
                     reads=[best], writes=[ex])
                kb.act(ex[:], ex[:], AF.Exp, reads=[ex], writes=[ex])
                kb.v("tensor_reduce", sm[:], ex[:], AX.X, ALU.add, reads=[ex], writes=[sm])
                kb.v("reciprocal", sm[:], sm[:], reads=[sm], writes=[sm])
                kb.v("tensor_tensor", sel[:, 2, :].rearrange("p (h m) -> p h m", m=16), ex[:],
                     sm[:].unsqueeze(2).broadcast_to([128, 8, 16]), ALU.mult, reads=[ex, sm], writes=[(sel, 2)])
                if "SEL" in self.debug:
                    kb.dma(self.SEL[tile_i * 128:(tile_i + 1) * 128], sel[:], reads=[sel], writes=[(self.SEL, tile_i)])
                for j in range(3):
                    kb.tr(ps_t[:, j, :], sel[:, j, :], self.C(CST_IDENT), reads=[sel, self.cstsb], writes=[(ps_t, j)])
                kb.act(sT[:], ps_t[:], AF.Copy, reads=[ps_t], writes=[sT])

        def back(tile_i):
            if True:
                sT = selT[tile_i % 2]
                WD = WDsb[tile_i % 2]
                for qq in range(128 // TQ):
                    A, B = Abig[cn['ab'] % 2], Bbig[cn['ab'] % 2]
                    cn['ab'] += 1
                    ts_ = slice(qq * TQ, (qq + 1) * TQ)
                    kb.v("tensor_tensor", A[:], iota3, sT[:, 0, ts_].unsqueeze(2).broadcast_to([128, TQ, 128]), ALU.is_equal,
                         reads=[sT, self.cstsb], writes=[A])
                    kb.v("tensor_tensor", A[:], A[:], sT[:, 2, ts_].unsqueeze(2).broadcast_to([128, TQ, 128]), ALU.mult,
                         reads=[A, sT], writes=[A], eng="gpsimd")
                    kb.v("tensor_tensor", B[:], iota3, sT[:, 1, ts_].unsqueeze(2).broadcast_to([128, TQ, 128]), ALU.is_equal,
                         reads=[sT, self.cstsb], writes=[B])
                    for q4 in range(TQ // 4):
                        pw = ps_w[cn['wi'] % 2]
                        cn['wi'] += 1
                        for tq in range(4):
                            t = q4 * 4 + tq
                            kb.mm(pw[:, tq, :], B[:, t, :], A[:, t, :], True, True, reads=[A, B], writes=[pw])
                        tb = (tile_i % 2) * 128 + qq * TQ + q4 * 4
                        kb.act(WD[:, :, tb:tb + 4], pw[:].rearrange("p t k -> p k t"), AF.Copy, reads=[pw],
                               writes=[(WD, tb)])
                if tile_i % 2 == 1:
                    pr = tile_i // 2
                    kb.dma(self.WDs[:, :, pr * 256:(pr + 1) * 256].rearrange("a b t -> b a t"), WD[:], reads=[WD],
                           writes=[(self.WDs, pr)])

        for tg in range(9):
            h = hb[0]
            t0 = tg * 512
            kb.dma(h[:], self.HT[:, t0:t0 + 512].rearrange("(c p) t -> p c t", p=128), reads=[self.HT], writes=[h])
            for fc in range(16):
                w = wq[fc % 2]
                kb.dma(w[:], self.WQ[l][:, fc * 128:(fc + 1) * 128].rearrange("(c p) f -> p c f", p=128),
                       reads=[self.WQ[l]], writes=[w])
                for c in range(16):
                    kb.mm(ps_q[:], w[:, c, :], h[:, c, :], c == 0, c == 15, reads=[w, h], writes=[ps_q])
                kb.act(qT[:, fc, :], ps_q[:], AF.Copy, reads=[ps_q], writes=[(qT, fc)])
            for tt in range(4):
                tile_i = tg * 4 + tt
                front(tg, tt)
                if tile_i >= 1:
                    back(tile_i - 1)
        back(NTILE - 1)
        kb.release(m0)

    def stage_peer_dense(self, l, xin, xout):
        kb = self.kb
        m0 = kb.mark()
        TP = 768
        NSUB = 2
        SUB = TP // NSUB
        NTT = TP // 128
        g2 = [kb.sb("pd_g2%d" % r, [128, D], F32) for r in range(1)]
        hp = kb.sb("pd_h", [128, 16, TP], BF16)
        acc = kb.sb("pd_acc", [128, NTT, D], F32)
        ut = [kb.sb("pd_ut%d" % i, [128, 16, 512], BF16) for i in range(2)]
        vg = [kb.sb("pd_vg%d" % i, [128, 4, D], BF16) for i in range(2)]
        wd = [kb.sb("pd_wd%d" % i, [128, 4, TP], BF16) for i in range(2)]
        GT = [kb.sb("pd_GT%d" % i, [128, 4, TP], BF16) for i in range(2)]
        ge = [kb.sb("pd_ge%d" % i, [128, SUB], BF16) for i in range(2)]
        xt = [kb.sb("pd_x%d" % i, [128, D], F32) for i in range(2)]
        ps_u = [kb.ps("pd_pu%d" % i, [128, SUB]) for i in range(2)]
        ps_v = [kb.ps("pd_pv%d" % i, [128, 512]) for i in range(4)]
        gi = 0
        vi = 0
        NP = NT // TP
        work = [(p, eg) for p in range(NP) for eg in range(32)]

        def load(i):
            p, eg = work[i]
            j = i % 2
            e0 = eg * 512
            t0 = p * TP
            kb.dma(ut[j][:], self.UTB[l][:, e0:e0 + 512].rearrange("(c p) e -> p c e", p=128), reads=[self.UTB[l]],
                   writes=[ut[j]])
            kb.dma(vg[j][:], self.VB[l][e0:e0 + 512, :].rearrange("(a b) d -> b a d", b=128), reads=[self.VB[l]],
                   writes=[vg[j]])
            kb.dma(wd[j][:], self.WDs[eg * 4:(eg + 1) * 4, :, t0:t0 + TP].rearrange("a b t -> b a t"),
                   reads=[self.WDs], writes=[wd[j]])
        load(0)
        for i, (p, eg) in enumerate(work):
            t0 = p * TP
            j = i % 2
            if eg == 0:
                kb.dma(hp[:], self.HT[:, t0:t0 + TP].rearrange("(c p) t -> p c t", p=128), reads=[self.HT], writes=[hp])
                kb.v("memset", acc[:], 0.0, writes=[acc], eng="gpsimd")
            if i + 1 < len(work):
                load(i + 1)
            G = GT[j]
            for k1 in range(4):
                for sb_ in range(NSUB):
                    pu = ps_u[gi % 2]
                    g = ge[gi % 2]
                    gi += 1
                    ts_ = slice(sb_ * SUB, (sb_ + 1) * SUB)
                    for c in range(16):
                        kb.mm(pu[:], ut[j][:, c, k1 * 128:(k1 + 1) * 128], hp[:, c, ts_], c == 0, c == 15,
                              reads=[ut[j], hp], writes=[pu])
                    kb.act(g[:], pu[:], AF.Gelu, reads=[pu], writes=[g])
                    kb.v("tensor_tensor", G[:, k1, ts_], g[:], wd[j][:, k1, ts_], ALU.mult, reads=[g, wd[j]],
                         writes=[(G, (k1, sb_))], eng="gpsimd")
            for tt in range(NTT):
                for nb in range(4):
                    pv = ps_v[vi % 4]
                    vi += 1
                    for k1 in range(4):
                        kb.mm(pv[:], G[:, k1, tt * 128:(tt + 1) * 128], vg[j][:, k1, nb * 512:(nb + 1) * 512],
                              k1 == 0, k1 == 3, reads=[G, vg[j]], writes=[pv])
                    cs = slice(nb * 512, (nb + 1) * 512)
                    kb.v("tensor_tensor", acc[:, tt, cs], acc[:, tt, cs], pv[:], ALU.add, reads=[(acc, (tt, nb)), pv],
                         writes=[(acc, (tt, nb))])
            if eg == 31:
                for tt in range(NTT):
                    x = xt[tt % 2]
                    tok0 = t0 + tt * 128
                    r = 0 if tok0 < 2048 else (1 if tok0 < 4096 else 2)
                    rows = slice(tok0, tok0 + 128)
                    if tt == 0 or tok0 in (2048, 4096):
                        kb.dma(g2[0][:], self.modd[r:r + 1, 5 * D:6 * D].broadcast_to([128, D]), reads=[self.modd],
                               writes=[g2[0]])
                    kb.dma(x[:], xin[rows, :], reads=[xin], writes=[x])
                    kb.v("tensor_tensor", acc[:, tt, :], acc[:, tt, :], g2[0][:], ALU.mult, reads=[acc, g2[0]], writes=[acc])
                    kb.v("tensor_tensor", x[:], x[:], acc[:, tt, :], ALU.add, reads=[x, acc], writes=[x])
                    kb.dma(xout[rows, :], x[:], reads=[x], writes=[(xout, (p, tt))])
        kb.release(m0)

    def build(self):
        kb = self.kb
        self.declare()
        self.load_consts()
        self.epsb = kb.sb("epsb", [128, 1], F32)
        kb.v("memset", self.epsb[:], EPS, writes=[self.epsb])
        kb.cur_stage = "prologue"
        kb.profile_scopes = self.profile
        self.prologue()
        st = self.stages
        xcur = self.x_in

        def S(name):
            kb.cur_stage = name
        for l in range(self.nlayers):
            if st is None or "mod" in st:
                S("mod%d" % l)
                self.stage_mod(l)
            if st is None or "norm1" in st:
                S("norm1_%d" % l)
                self.stage_norm(xcur, self.norm1_g[l:l + 1, :], 0, 1, self.HT)
            if st is None or "proj" in st:
                S("proj%d" % l)
                self.layer_casts(l)
                self.stage_proj(l)
            if st is None or "attn" in st:
                S("attn%d" % l)
                self.stage_attn(l)
            if st is None or "ssm" in st:
                S("ssmconv%d" % l)
                self.stage_ssm_conv(l)
                S("ssmscan%d" % l)
                self.stage_ssm_scan(l)
                S("ssmout%d" % l)
                self.stage_ssm_out(l)
            if st is None or "conf" in st:
                S("conf%d" % l)
                self.stage_conf(l)
            if st is None or "merge" in st:
                S("merge%d" % l)
                self.stage_merge(l, xcur, self.xs[0])
            if st is None or "peer" in st:
                S("norm2_%d" % l)
                self.stage_norm(self.xs[0], self.norm2_g[l:l + 1, :], 3, 4, self.HT)
                S("route%d" % l)
                self.stage_peer_route(l)
                S("dense%d" % l)
                self.stage_peer_dense(l, self.xs[0], self.xs[1])
                xcur = self.xs[1]
        S("final")
        if st is None or "final" in st:
            self.stage_norm(xcur, self.final_g[0:1, :], 0, 0, None, final_out=self.out)
        kb.barrier()
        return kb.build(final_waits=self.finals)


def host_inputs(inputs, core):
    b0, b1 = 2 * core, 2 * core + 1
    x = inputs["x"]
    ctx = inputs["ctx"]
    m = {}
    m["x_c"] = np.ascontiguousarray(np.concatenate([x[b0], x[b1], ctx[b0], ctx[b1]], axis=0))
    m["c3T"] = np.ascontiguousarray(np.stack([inputs["c"][b0], inputs["c"][b1], inputs["c_ctx"]], axis=1))
    return m


def shared_inputs(inputs):
    m = {}
    m["cst"] = host_consts()
    for k in ("w_mod", "b_mod", "norm1_g", "norm2_g", "w_in"):
        m[k] = np.ascontiguousarray(inputs[k])
    m["final_norm_g"] = np.ascontiguousarray(inputs["final_norm_g"].reshape(1, D))
    m["biasT"] = host_bias_tables(inputs["na_rpb"])
    m["ssm_cwT"] = np.ascontiguousarray(inputs["ssm_conv_w"].transpose(0, 2, 1))
    m["ssm_conv_b"] = np.ascontiguousarray(inputs["ssm_conv_b"])
    m["ssm_dtb"] = np.ascontiguousarray(inputs["ssm_dt_bias"].reshape(2, 32))
    m["ssm_Alog"] = np.ascontiguousarray(inputs["ssm_A_log"].reshape(2, 32))
    m["ssm_Dx"] = np.ascontiguousarray(np.repeat(inputs["ssm_D"], 64, axis=1))
    m["ssm_norm_g"] = np.ascontiguousarray(inputs["ssm_norm_g"])
    m["cv_wT"] = np.ascontiguousarray(inputs["cv_dw_w"].transpose(0, 2, 1))
    m["keysT"] = np.ascontiguousarray(inputs["peer_keys"].reshape(2, 16, 128, 128).transpose(0, 3, 1, 2))
    m["peer_uT"] = np.ascontiguousarray(inputs["peer_u"].transpose(0, 2, 1))
    for k in ("cv_dw_b", "cv_ln_g", "cv_ln_b", "cv_bo", "na_wo", "ssm_wo", "cv_wo", "w_out", "peer_wq", "peer_v"):
        m[k] = np.ascontiguousarray(inputs[k])
    return m


def run(inputs, cores=8, debug=(), nlayers=2, stages=None, trace=False, profile=False):
    p = Prog(debug=debug, nlayers=nlayers, stages=stages, profile=profile)
    nc = p.build()
    sh = shared_inputs(inputs)
    in_maps = []
    for c in range(cores):
        m = dict(sh)
        m.update(host_inputs(inputs, c))
        in_maps.append(m)
    res = run_bass_kernel_spmd(nc, in_maps, core_ids=list(range(cores)), trace=trace)
    return res


def kernel(**inputs):
    inputs = {k: np.asarray(v) for k, v in inputs.items()}
    res = run(inputs)
    out = np.empty((16, 2048, D), np.float32)
    for c in range(8):
        o = res.results[c]["out"]
        out[2 * c] = o[:2048]
        out[2 * c + 1] = o[2048:]
    return out
```

```python
import numpy as np
import ml_dtypes
import concourse.bass as bass
import concourse.mybir as mybir
from concourse.bass_utils import run_bass_kernel_spmd

F32 = mybir.dt.float32
BF16 = mybir.dt.bfloat16
I32 = mybir.dt.int32
U32 = mybir.dt.uint32
AF = mybir.ActivationFunctionType
ALU = mybir.AluOpType
AX = mybir.AxisListType

ENGS = ("tensor", "vector", "scalar", "gpsimd", "sync")
N_DMA_SEMS = 6

D = 2048
NT = 4608
NTILE = 36
INC = 13856
EPS = 1e-6
SEGS = [(0, 2048, 0), (2048, 2048, 1), (4096, 256, 2), (4352, 256, 2)]
NEG = -30000.0


class Tile:
    def __init__(self, t, name):
        self.t = t
        self.name = name
        self.st = {}

    def __getitem__(self, idx):
        return self.t[idx]


class Op:
    __slots__ = ("eng", "fn", "waits", "signal", "idx", "is_dma", "dma_slot", "dma_val", "sigval", "stage")

    def __init__(self, eng, fn, is_dma=False):
        self.eng = eng
        self.fn = fn
        self.waits = []
        self.signal = False
        self.is_dma = is_dma
        self.dma_slot = None
        self.dma_val = None
        self.sigval = None
        self.stage = None


class KB:
    def __init__(self, same_engine_sync=True):
        self.nc = bass.Bass("TRN2", target_bir_lowering=False)
        self.ops = {e: [] for e in ENGS}
        self.same_engine_sync = same_engine_sync
        self._ctx = []
        self.all_ops = []
        self.pending_dmas = []
        self.events = []

    def dram(self, name, shape, dtype, kind="Internal"):
        t = self.nc.dram_tensor(name, list(shape), dtype, kind=kind)
        return Tile(t.ap(), name)

    def sb(self, name, shape, dtype):
        self._uid = getattr(self, "_uid", 0) + 1
        name = "%s_%d" % (name, self._uid)
        g = self.nc.sbuf_tensor(name, list(shape), dtype)
        t = g.__enter__()
        self._ctx.append(g)
        return Tile(t, name)

    def ps(self, name, shape, dtype=F32):
        self._uid = getattr(self, "_uid", 0) + 1
        name = "%s_%d" % (name, self._uid)
        g = self.nc.psum_tensor(name, list(shape), dtype)
        t = g.__enter__()
        self._ctx.append(g)
        return Tile(t, name)

    def mark(self):
        return len(self._ctx)

    def release(self, mark):
        self.barrier()
        while len(self._ctx) > mark:
            g = self._ctx.pop()
            g.__exit__(None, None, None)

    @staticmethod
    def _norm(x):
        if isinstance(x, tuple):
            return x[0], x[1]
        return x, None

    def _deps(self, op, reads, writes):
        deps = []
        for x in reads:
            t, k = self._norm(x)
            for kk, st in t.st.items():
                if k is None or kk is None or kk == k:
                    if st[0] is not None:
                        deps.append(st[0])
        for x in writes:
            t, k = self._norm(x)
            for kk, st in t.st.items():
                if k is None or kk is None or kk == k:
                    if st[0] is not None:
                        deps.append(st[0])
                    deps.extend(st[1])
        for x in reads:
            t, k = self._norm(x)
            st = t.st.setdefault(k, [None, []])
            st[1].append(op)
        for x in writes:
            t, k = self._norm(x)
            if k is None:
                t.st = {None: [op, []]}
            else:
                t.st[k] = [op, []]
        return deps

    def op(self, eng, fn, reads=(), writes=(), is_dma=False, nobarrier=False):
        o = Op(eng, fn, is_dma)
        o.stage = getattr(self, "cur_stage", None)
        deps = self._deps(o, reads, writes)
        seen = set()
        for d in deps:
            if d is o or id(d) in seen:
                continue
            seen.add(id(d))
            if d.eng == eng and not d.is_dma and not is_dma:
                if not self.same_engine_sync:
                    continue
                if eng == "tensor":
                    continue
            o.waits.append(d)
        o.idx = len(self.ops[eng])
        self.ops[eng].append(o)
        self.all_ops.append(o)
        if is_dma and not nobarrier:
            self.pending_dmas.append(o)
        return o

    def barrier(self):
        lasts = []
        for e in ENGS:
            for o in reversed(self.ops[e]):
                if not o.is_dma and o.fn is not None:
                    lasts.append(o)
                    break
        dmas = list(self.pending_dmas)
        self.pending_dmas = []
        for e in ENGS:
            o = Op(e, None)
            o.waits = [d for d in lasts if d.eng != e] + dmas
            o.idx = len(self.ops[e])
            self.ops[e].append(o)
            self.all_ops.append(o)

    def dma(self, out, in_, reads=(), writes=(), q="sync", nobarrier=False, **kw):
        def fn(e, out=out, in_=in_, kw=kw):
            return e.dma_start(out=out, in_=in_, **kw)
        return self.op(q, fn, reads, writes, is_dma=True, nobarrier=nobarrier)

    def mm(self, out, lhsT, rhs, start, stop, reads=(), writes=(), **kw):
        def fn(e):
            return e.matmul(out, lhsT, rhs, start=start, stop=stop, **kw)
        return self.op("tensor", fn, reads, writes)

    def tr(self, out, in_, ident, reads=(), writes=()):
        def fn(e):
            return e.transpose(out, in_, ident)
        return self.op("tensor", fn, reads, writes)

    def v(self, method, *args, reads=(), writes=(), eng="vector", **kw):
        def fn(e):
            return getattr(e, method)(*args, **kw)
        return self.op(eng, fn, reads, writes)

    def act(self, out, in_, func, reads=(), writes=(), **kw):
        def fn(e):
            return e.activation(out=out, in_=in_, func=func, **kw)
        return self.op("scalar", fn, reads, writes)

    def build(self, final_waits=()):
        nc = self.nc
        nslots = {e: 0 for e in ENGS}
        for e in ENGS:
            j = 0
            for o in self.ops[e]:
                if o.is_dma:
                    o.dma_slot = j % N_DMA_SEMS
                    o.dma_val = 16 * (j // N_DMA_SEMS + 1)
                    j += 1
            nslots[e] = min(j, N_DMA_SEMS)
        for o in self.all_ops:
            for d in o.waits:
                d.signal = True
        for o in final_waits:
            o.signal = True
        for e in ENGS:
            c = 0
            for o in self.ops[e]:
                if o.signal and not o.is_dma:
                    c += 1
                    o.sigval = c
        sems = {}
        ctxs = []

        def mksem(name):
            g = nc.semaphore(name)
            s = g.__enter__()
            ctxs.append(g)
            return s
        for e in ENGS:
            sems[e] = mksem("s_" + e)
            for j in range(nslots[e]):
                sems[(e, j)] = mksem("d_%s_%d" % (e, j))

        def semval(d):
            if d.is_dma:
                return sems[(d.eng, d.dma_slot)], d.dma_val
            return sems[d.eng], d.sigval

        blk = nc.Block()
        block = blk.__enter__()

        def run_engine(ename, eng):
            known = {}
            cur = [None, None]

            def set_scope(name):
                if not getattr(self, "profile_scopes", False) or name == cur[0]:
                    return
                if cur[1] is not None:
                    cur[1].__exit__(None, None, None)
                    cur[1] = None
                cur[0] = name
                if name is not None:
                    cur[1] = nc.named_scope(name)
                    cur[1].__enter__()
            for o in self.ops[ename]:
                if o.fn is not None:
                    set_scope(o.stage)
                wl = {}
                for d in o.waits:
                    s, v = semval(d)
                    key = id(s)
                    if known.get(key, 0) >= v:
                        continue
                    if key not in wl or wl[key][1] < v:
                        wl[key] = (s, v)
                if o.is_dma:
                    s = sems[(ename, o.dma_slot)]
                    pv = o.dma_val - 16
                    if pv > 0 and known.get(id(s), 0) < pv:
                        if id(s) not in wl or wl[id(s)][1] < pv:
                            wl[id(s)] = (s, pv)
                for key, (s, v) in wl.items():
                    eng.wait_ge(s, v)
                    known[key] = v
                if o.fn is None:
                    continue
                ins = o.fn(eng)
                if o.is_dma:
                    ins.then_inc(sems[(ename, o.dma_slot)], 16)
                elif o.signal:
                    ins.then_inc(sems[ename], 1)
            set_scope(None)
            if ename == "sync":
                for d in final_waits:
                    s, v = semval(d)
                    eng.wait_ge(s, v)

        @block.sync
        def _(e):
            run_engine("sync", e)

        @block.tensor
        def _(e):
            run_engine("tensor", e)

        @block.vector
        def _(e):
            run_engine("vector", e)

        @block.scalar
        def _(e):
            run_engine("scalar", e)

        @block.gpsimd
        def _(e):
            run_engine("gpsimd", e)

        blk.__exit__(None, None, None)
        for g in reversed(ctxs):
            g.__exit__(None, None, None)
        while self._ctx:
            self._ctx.pop().__exit__(None, None, None)
        return nc


CST_IDENT, CST_TRIF, CST_TRIB, CST_STRF, CST_STRB, CST_ONES, CST_IOTA = range(7)


def host_consts():
    k = np.arange(128)[:, None]
    t = np.arange(128)[None, :]
    mats = [
        np.eye(128),
        (k <= t), (k >= t), (k > t), (k < t),
        np.ones((128, 128)),
        np.broadcast_to(t, (128, 128)),
    ]
    return np.concatenate([m.astype(np.float32) for m in mats], axis=1)


def host_bias_tables(rpb):
    L = rpb.shape[0]
    out = np.empty((L, 16, 5, 640, 128), np.float32)
    kl = np.arange(640)[:, None]
    ql = np.arange(128)[None, :]
    for ty, t in enumerate([0, 1, 2, 14, 15]):
        ks = int(np.clip(2 * t - 4, 0, 22))
        rk = ks + kl // 64
        jk = kl % 64
        rq = 2 * t + ql // 64
        jq = ql % 64
        rs = np.clip(rq - 4, 0, 24)
        cs = np.clip(jq - 8, 0, 48)
        valid = (rk >= rs) & (rk < rs + 8) & (jk >= cs) & (jk < cs + 16)
        ri = np.clip(rk - rq + 7, 0, 14)
        ci = np.clip(jk - jq + 15, 0, 30)
        g = rpb[:, :, ri, ci]
        out[:, :, ty] = np.where(valid[None, None], g, np.float32(NEG))
    return out


def tile_type(t):
    return {0: 0, 1: 1, 14: 3, 15: 4}.get(t, 2)


class Prog:
    def __init__(self, debug=(), nlayers=2, stages=None, profile=False):
        self.profile = profile
        self.kb = KB()
        self.debug = set(debug)
        self.nlayers = nlayers
        self.stages = stages
        self.finals = []

    def scratch(self, name, shape, dtype):
        kind = "ExternalOutput" if name in self.debug else "Internal"
        return self.kb.dram(name, shape, dtype, kind)

    def inp(self, name, shape, dtype=F32):
        return self.kb.dram(name, shape, dtype, "ExternalInput")

    def declare(self):
        L = 2
        s = self
        s.x_in = s.inp("x_c", [NT, D])
        s.c3T = s.inp("c3T", [D, 3])
        s.cst = s.inp("cst", [128, 7 * 128])
        s.w_mod = s.inp("w_mod", [L, D, 6 * D])
        s.b_mod = s.inp("b_mod", [L, 6 * D])
        s.norm1_g = s.inp("norm1_g", [L, D])
        s.norm2_g = s.inp("norm2_g", [L, D])
        s.w_in = s.inp("w_in", [L, D, INC])
        s.final_g = s.inp("final_norm_g", [1, D])
        s.biasT = s.inp("biasT", [L, 16, 5, 640, 128])
        s.AO = s.scratch("AO", [NT, 1024], BF16)
        s.ssm_cwT = s.inp("ssm_cwT", [L, 1536, 5])
        s.ssm_cb = s.inp("ssm_conv_b", [L, 1536])
        s.ssm_dtb = s.inp("ssm_dtb", [L, 32])
        s.ssm_Alog = s.inp("ssm_Alog", [L, 32])
        s.ssm_Dx = s.inp("ssm_Dx", [L, 1024])
        s.ssm_ng = s.inp("ssm_norm_g", [L, 1024])
        s.cv_wT = s.inp("cv_wT", [L, 1024, 31])
        s.cv_dw_b = s.inp("cv_dw_b", [L, 1024])
        s.cv_ln_g = s.inp("cv_ln_g", [L, 1024])
        s.cv_ln_b = s.inp("cv_ln_b", [L, 1024])
        s.cv_bo = s.inp("cv_bo", [L, 2048])
        s.na_wo = s.inp("na_wo", [L, 1024, D])
        s.ssm_wo = s.inp("ssm_wo", [L, 1024, D])
        s.cv_wo = s.inp("cv_wo", [L, 1024, D])
        s.w_out = s.inp("w_out", [L, D, D])
        s.NAW = [s.scratch("NAW%d" % l, [1024, D], BF16) for l in range(L)]
        s.SSW = [s.scratch("SSW%d" % l, [1024, D], BF16) for l in range(L)]
        s.CVW = [s.scratch("CVW%d" % l, [1024, D], BF16) for l in range(L)]
        s.WO = [s.scratch("WO%d" % l, [D, D], BF16) for l in range(L)]
        s.CVAT = s.scratch("CVAT", [1024, NT], BF16)
        s.peer_wq = s.inp("peer_wq", [L, D, D])
        s.keysT = s.inp("keysT", [L, 128, 16, 128])
        s.peer_uT = s.inp("peer_uT", [L, D, 16384])
        s.peer_v = s.inp("peer_v", [L, 16384, D])
        s.WQ = [s.scratch("WQ%d" % l, [D, D], BF16) for l in range(L)]
        s.UTB = [s.scratch("UTB%d" % l, [D, 16384], BF16) for l in range(L)]
        s.VB = [s.scratch("VB%d" % l, [16384, D], BF16) for l in range(L)]
        s.WDs = s.scratch("WDs", [128, 128, NT], BF16)
        s.SEL = s.scratch("SEL", [NT, 3, 128], F32)
        s.XBCc = s.scratch("XBCc", [512, NT], BF16)
        s.XTOK = s.scratch("XTOK", [NT, 1280], BF16)
        s.YD = [s.scratch("YD%d" % i, [NT, 1024], F32) for i in range(2)]
        s.YST = s.scratch("YST", [1024, NT], BF16)
        s.out = s.kb.dram("out", [4096, D], F32, "ExternalOutput")
        s.modd = s.scratch("modd", [3, 6 * D], F32)
        s.HT = s.scratch("HT", [D, NT], BF16)
        s.Wi = [s.scratch("Wi%d" % l, [D, INC], BF16) for l in range(L)]
        s.QT = s.scratch("QT", [1024, NT], BF16)
        s.KT = s.scratch("KT", [1024, NT], BF16)
        s.Vt = s.scratch("Vt", [NT, 1024], BF16)
        s.Zt = s.scratch("Zt", [NT, 1024], BF16)
        s.XBCT = s.scratch("XBCT", [1536, NT], BF16)
        s.DTt = s.scratch("DTt", [NT, 32], F32)
        s.GLUT = s.scratch("GLUT", [2048, NT], BF16)
        s.GATET = s.scratch("GATET", [6144, NT], BF16)
        s.xs = [s.scratch("xs%d" % i, [NT, D], F32) for i in range(2)]

    def load_consts(self):
        kb = self.kb
        self.cstsb = kb.sb("cstsb", [128, 7 * 128], F32)
        kb.dma(self.cstsb[:], self.cst[:], writes=[self.cstsb])
        self.identb = kb.sb("identb", [128, 128], BF16)
        kb.v("tensor_copy", self.identb[:], self.cstsb[:, 0:128], reads=[self.cstsb], writes=[self.identb])

    def C(self, i):
        return self.cstsb[:, i * 128:(i + 1) * 128]

    def cast_dram(self, dst, src_ap, rows, rows_per=512):
        kb = self.kb
        for i in range(0, rows, rows_per):
            r = min(rows_per, rows - i)
            kb.dma(dst[i:i + r, :], src_ap[i:i + r, :], writes=[(dst, ("c", i))], q="gpsimd", nobarrier=True)

    def prologue(self):
        self.cast_dram(self.Wi[0], self.w_in[0], D)

    def layer_casts(self, l):
        self.cast_dram(self.NAW[l], self.na_wo[l], 1024, 1024)
        self.cast_dram(self.SSW[l], self.ssm_wo[l], 1024, 1024)
        self.cast_dram(self.CVW[l], self.cv_wo[l], 1024, 1024)
        self.cast_dram(self.WO[l], self.w_out[l], D, 1024)
        self.cast_dram(self.WQ[l], self.peer_wq[l], D, 1024)
        self.cast_dram(self.UTB[l], self.peer_uT[l], D, 128)
        self.cast_dram(self.VB[l], self.peer_v[l], 16384, 1024)
        if l + 1 < self.nlayers:
            self.cast_dram(self.Wi[l + 1], self.w_in[l + 1], D)

    def stage_mod(self, l):
        kb = self.kb
        m0 = kb.mark()
        c3 = kb.sb("c3", [128, 16, 3], F32)
        kb.dma(c3[:], self.c3T[:].rearrange("(c p) r -> p c r", p=128), writes=[c3])
        c3s = kb.sb("c3s", [128, 16, 3], F32)
        kb.act(c3s[:], c3[:], AF.Silu, reads=[c3], writes=[c3s])
        wbuf = [kb.sb("wm%d" % i, [128, 16, 512], F32) for i in range(2)]
        bmb = [kb.sb("bm%d" % i, [3, 512], F32) for i in range(2)]
        pss = [kb.ps("pm%d" % i, [3, 512]) for i in range(2)]
        ob = [kb.sb("om%d" % i, [3, 512], F32) for i in range(2)]
        for nb in range(24):
            w = wbuf[nb % 2]
            bm = bmb[nb % 2]
            ps = pss[nb % 2]
            o = ob[nb % 2]
            kb.dma(w[:], self.w_mod[l, :, nb * 512:(nb + 1) * 512].rearrange("(c p) n -> p c n", p=128), writes=[w])
            kb.dma(bm[:], self.b_mod[l:l + 1, nb * 512:(nb + 1) * 512].broadcast_to([3, 512]), writes=[bm])
            for c in range(16):
                kb.mm(ps[:], c3s[:, c, :], w[:, c, :], c == 0, c == 15, reads=[c3s, w], writes=[ps])
            kb.v("tensor_tensor", o[:], ps[:], bm[:], ALU.add, reads=[ps, bm], writes=[o])
            kb.dma(self.modd[:, nb * 512:(nb + 1) * 512], o[:], reads=[o], writes=[(self.modd, nb)])
        kb.release(m0)

    def stage_norm(self, xin, gain_ap, shift_col, scale_col, HT, final_out=None):
        kb = self.kb
        m0 = kb.mark()
        gb = kb.sb("gb", [128, D], F32)
        kb.dma(gb[:], gain_ap.broadcast_to([128, D]), writes=[gb])
        A = []
        Bt = []
        if final_out is None:
            for r in range(3):
                a = kb.sb("nA%d" % r, [128, D], F32)
                b = kb.sb("nB%d" % r, [128, D], F32)
                kb.dma(a[:], self.modd[r:r + 1, scale_col * D:(scale_col + 1) * D].broadcast_to([128, D]),
                       reads=[self.modd], writes=[a])
                kb.dma(b[:], self.modd[r:r + 1, shift_col * D:(shift_col + 1) * D].broadcast_to([128, D]),
                       reads=[self.modd], writes=[b])
                kb.v("scalar_tensor_tensor", a[:], a[:], 1.0, gb[:], ALU.add, ALU.mult, reads=[a, gb], writes=[a])
                A.append(a)
                Bt.append(b)
        xt = [kb.sb("nx%d" % i, [128, D], F32) for i in range(2)]
        junk = kb.sb("njunk", [128, D], F32)
        ss = [kb.sb("nss%d" % i, [128, 1], F32) for i in range(2)]
        rs = [kb.sb("nrs%d" % i, [128, 1], F32) for i in range(2)]
        y32 = [kb.sb("ny32%d" % i, [128, D], F32) for i in range(2)]
        if final_out is None:
            yb = [kb.sb("nyb%d" % i, [128, D], BF16) for i in range(2)]
            pst = [kb.ps("npt%d" % i, [128, 16, 128], BF16) for i in range(2)]
            hT = [kb.sb("nhT%d" % i, [128, 16, 128], BF16) for i in range(2)]
        ntiles = NTILE if final_out is None else 32
        for t in range(ntiles):
            i = t % 2
            r = 0 if t < 16 else (1 if t < 32 else 2)
            kb.dma(xt[i][:], xin[t * 128:(t + 1) * 128, :], reads=[(xin, t)], writes=[xt[i]])
            kb.act(junk[:], xt[i][:], AF.Square, reads=[xt[i]], writes=[junk, ss[i]], accum_out=ss[i][:])
            kb.act(rs[i][:], ss[i][:], AF.Sqrt, reads=[ss[i]], writes=[rs[i]], scale=1.0 / D, bias=self.epsb[:])
            kb.v("reciprocal", rs[i][:], rs[i][:], reads=[rs[i]], writes=[rs[i]])
            if final_out is not None:
                kb.v("scalar_tensor_tensor", y32[i][:], xt[i][:], rs[i][:], gb[:], ALU.mult, ALU.mult,
                     reads=[xt[i], rs[i], gb], writes=[y32[i]])
                o = kb.dma(final_out[t * 128:(t + 1) * 128, :], y32[i][:], reads=[y32[i]], writes=[(final_out, t)])
                self.finals.append(o)
                continue
            kb.v("scalar_tensor_tensor", y32[i][:], xt[i][:], rs[i][:], A[r][:], ALU.mult, ALU.mult,
                 reads=[xt[i], rs[i], A[r]], writes=[y32[i]])
            kb.v("tensor_tensor", yb[i][:], y32[i][:], Bt[r][:], ALU.add, reads=[y32[i], Bt[r]], writes=[yb[i]], eng="gpsimd")
            for c in range(16):
                kb.tr(pst[i][:, c, :], yb[i][:, c * 128:(c + 1) * 128], self.identb[:],
                      reads=[yb[i], self.identb], writes=[(pst[i], c)])
            kb.act(hT[i][:], pst[i][:], AF.Copy, reads=[pst[i]], writes=[hT[i]])
            kb.dma(HT[:, t * 128:(t + 1) * 128].rearrange("(c p) t -> p c t", p=128), hT[i][:],
                   reads=[hT[i]], writes=[(HT, t)])
        kb.release(m0)

    def stage_proj(self, l):
        kb = self.kb
        m0 = kb.mark()
        Wi = self.Wi[l]
        secs = [(self.QT, "f", 0, 1024), (self.KT, "f", 1024, 1024), (self.Vt, "t", 2048, 1024),
                (self.Zt, "t", 3072, 1024), (self.XBCT, "f", 4096, 1536), (self.DTt, "t", 5632, 32),
                (self.GLUT, "f", 5664, 2048), (self.GATET, "f", 7712, 6144)]
        blocks = []
        for dst, kind, c0, n in secs:
            for j in range(0, n, 512):
                blocks.append((dst, kind, c0 + j, j, min(512, n - j)))
        TG = 1536
        hbuf = [kb.sb("ph%d" % i, [128, 16, TG], BF16) for i in range(2)]
        wbuf = [kb.sb("pw%d" % i, [128, 16, 512], BF16) for i in range(3)]
        pss = [kb.ps("pp%d" % i, [128, 512]) for i in range(4)]
        obf = [kb.sb("pob%d" % i, [128, 512], BF16) for i in range(4)]
        o32 = [kb.sb("po32%d" % i, [128, 32], F32) for i in range(2)]
        NG = NT // TG
        items = [(tg, blk) for tg in range(NG) for blk in blocks]

        def load_h(tg):
            kb.dma(hbuf[tg % 2][:], self.HT[:, tg * TG:(tg + 1) * TG].rearrange("(c p) t -> p c t", p=128),
                   reads=[self.HT], writes=[hbuf[tg % 2]])

        def load_w(i):
            tg, (dst, kind, wc0, dc0, n) = items[i]
            kb.dma(wbuf[i % 3][:, :, 0:n], Wi[:, wc0:wc0 + n].rearrange("(c p) n -> p c n", p=128), reads=[Wi],
                   writes=[wbuf[i % 3]])
        load_h(0)
        load_w(0)
        load_w(1)
        cnt = 0
        for i, (tg, (dst, kind, wc0, dc0, n)) in enumerate(items):
            if i + 2 < len(items):
                load_w(i + 2)
            if i % len(blocks) == 0 and tg + 1 < NG:
                load_h(tg + 1)
            h = hbuf[tg % 2]
            w = wbuf[i % 3]
            t0 = tg * TG
            if kind == "f":
                for sub in range(n // 128):
                    for tb in range(TG // 512):
                        ps = pss[cnt % 4]
                        ob = obf[cnt % 4]
                        cnt += 1
                        for c in range(16):
                            kb.mm(ps[:], w[:, c, sub * 128:(sub + 1) * 128], h[:, c, tb * 512:(tb + 1) * 512],
                                  c == 0, c == 15, reads=[w, h], writes=[ps])
                        kb.act(ob[:], ps[:], AF.Copy, reads=[ps], writes=[ob])
                        r0 = dc0 + sub * 128
                        c0 = t0 + tb * 512
                        kb.dma(dst[r0:r0 + 128, c0:c0 + 512], ob[:], reads=[ob], writes=[(dst, ("p", c0, r0))], q="scalar")
            else:
                for tt in range(TG // 128):
                    ps = pss[cnt % 4]
                    ob = obf[cnt % 4]
                    cnt += 1
                    for c in range(16):
                        kb.mm(ps[:, 0:n], h[:, c, tt * 128:(tt + 1) * 128], w[:, c, 0:n], c == 0, c == 15,
                              reads=[w, h], writes=[ps])
                    rows = slice(t0 + tt * 128, t0 + (tt + 1) * 128)
                    if n == 32:
                        o = o32[tt % 2]
                        kb.act(o[:], ps[:, 0:32], AF.Copy, reads=[ps], writes=[o])
                        kb.dma(dst[rows, :], o[:], reads=[o], writes=[(dst, ("p", tg, tt))], q="scalar")
                    else:
                        kb.act(ob[:], ps[:], AF.Copy, reads=[ps], writes=[ob])
                        kb.dma(dst[rows, dc0:dc0 + 512], ob[:], reads=[ob], writes=[(dst, ("p", tg, tt, dc0))], q="scalar")
        kb.release(m0)

    def stage_attn(self, l):
        kb = self.kb
        m0 = kb.mark()
        bias = [kb.sb("abias%d" % i, [128, 25, 128], F32) for i in range(2)]
        qts = [kb.sb("aq%d" % i, [64, 2304], BF16) for i in range(2)]
        kts = [kb.sb("ak%d" % i, [64, 2304], BF16) for i in range(2)]
        vas = [kb.sb("av%d" % i, [128, 18, 65], BF16) for i in range(2)]
        aos = [kb.sb("ao%d" % i, [128, 18, 64], BF16) for i in range(2)]
        tmps = [kb.sb("atmp%d" % i, [128, 640], F32) for i in range(3)]
        pTs = [kb.sb("apT%d" % i, [128, 896], BF16) for i in range(3)]
        rds = [kb.sb("ard%d" % i, [128, 1], F32) for i in range(3)]
        pss = [kb.ps("aps%d" % i, [128, 2, 512]) for i in range(3)]
        pso = [kb.ps("apo%d" % i, [128, 65]) for i in range(2)]
        for va in vas:
            kb.v("memset", va[:], 1.0, writes=[va])
        it = 0
        hb = 0
        for h in range(16):
            bt = bias[h % 2]
            kb.dma(bt[:], self.biasT[l, h].rearrange("ty (c k) q -> k (ty c) q", k=128), writes=[bt])
            for b in range(2):
                qt, kt, va, ao = qts[hb % 2], kts[hb % 2], vas[hb % 2], aos[hb % 2]
                hb += 1
                l0, c0t = b * 2048, 4096 + b * 256
                hs = slice(h * 64, (h + 1) * 64)
                kb.dma(qt[:, 0:2048], self.QT[hs, l0:l0 + 2048], reads=[self.QT], writes=[(qt, 0)])
                kb.dma(qt[:, 2048:2304], self.QT[hs, c0t:c0t + 256], reads=[self.QT], writes=[(qt, 1)])
                kb.dma(kt[:, 0:2048], self.KT[hs, l0:l0 + 2048], reads=[self.KT], writes=[(kt, 0)])
                kb.dma(kt[:, 2048:2304], self.KT[hs, c0t:c0t + 256], reads=[self.KT], writes=[(kt, 1)])
                kb.dma(va[:, 0:16, 0:64], self.Vt[l0:l0 + 2048, hs].rearrange("(c p) d -> p c d", p=128),
                       reads=[self.Vt], writes=[(va, 0)])
                kb.dma(va[:, 16:18, 0:64], self.Vt[c0t:c0t + 256, hs].rearrange("(c p) d -> p c d", p=128),
                       reads=[self.Vt], writes=[(va, 1)])
                for t in range(18):
                    i = it % 3
                    po = pso[it % 2]
                    it += 1
                    ps, tmp, pT, rd = pss[i], tmps[i], pTs[i], rds[i]
                    if t < 16:
                        cc0 = int(np.clip(2 * t - 4, 0, 22)) // 2
                        chunks = [cc0 + j for j in range(5)] + [16, 17]
                        ty = tile_type(t)
                    else:
                        chunks = [16, 17]
                    for j, ch in enumerate(chunks):
                        kb.mm(ps[:, j // 4, (j % 4) * 128:(j % 4 + 1) * 128], kt[:, ch * 128:(ch + 1) * 128],
                              qt[:, t * 128:(t + 1) * 128], True, True, reads=[kt, qt], writes=[ps])
                    if t < 16:
                        kb.v("scalar_tensor_tensor", tmp[:, 0:512], ps[:, 0, :], 0.125,
                             bt[:, ty * 5:ty * 5 + 4, :], ALU.mult, ALU.add, reads=[ps, bt], writes=[tmp])
                        kb.v("scalar_tensor_tensor", tmp[:, 512:640], ps[:, 1, 0:128], 0.125,
                             bt[:, ty * 5 + 4, :], ALU.mult, ALU.add, reads=[ps, bt], writes=[tmp])
                        kb.act(pT[:, 0:640], tmp[:], AF.Exp, reads=[tmp], writes=[pT])
                        kb.act(pT[:, 640:896], ps[:, 1, 128:384], AF.Exp, reads=[ps], writes=[pT], scale=0.125)
                    else:
                        kb.act(pT[:, 0:256], ps[:, 0, 0:256], AF.Exp, reads=[ps], writes=[pT], scale=0.125)
                    n = len(chunks)
                    for j, ch in enumerate(chunks):
                        kb.mm(po[:], pT[:, j * 128:(j + 1) * 128], va[:, ch, :], j == 0, j == n - 1,
                              reads=[pT, va], writes=[po])
                    kb.v("reciprocal", rd[:], po[:, 64:65], reads=[po], writes=[rd])
                    kb.v("tensor_scalar", ao[:, t, :], po[:, 0:64], rd[:], None, ALU.mult, reads=[po, rd], writes=[ao])
                kb.dma(self.AO[l0:l0 + 2048, hs].rearrange("(c p) d -> p c d", p=128), ao[:, 0:16, :],
                       reads=[ao], writes=[(self.AO, ("l", h, b))])
                kb.dma(self.AO[c0t:c0t + 256, hs].rearrange("(c p) d -> p c d", p=128), ao[:, 16:18, :],
                       reads=[ao], writes=[(self.AO, ("c", h, b))])
        kb.release(m0)

    def build_diags(self, name, wT_ap, nch, K):
        kb = self.kb
        wc = kb.sb(name + "_wc", [128, nch, K], F32)
        kb.dma(wc[:], wT_ap.rearrange("(c p) k -> p c k", p=128), writes=[wc])
        dgs = []
        for c in range(nch):
            dg = kb.sb("%s_dg%d" % (name, c), [128, K, 128], BF16)
            for k in range(K):
                kb.v("tensor_scalar", dg[:, k, :], self.C(CST_IDENT), wc[:, c, k:k + 1], None, ALU.mult,
                     reads=[wc, self.cstsb], writes=[(dg, k)], eng=("vector" if (c + k) % 2 == 0 else "gpsimd"))
            dgs.append(dg)
        return dgs

    def stage_ssm_conv(self, l):
        kb = self.kb
        m0 = kb.mark()
        dgs = self.build_diags("sc", self.ssm_cwT[l], 12, 5)
        cb = kb.sb("sc_cb", [128, 12], F32)
        kb.dma(cb[:], self.ssm_cb[l].rearrange("(c p) -> p c", p=128), writes=[cb], allow_slow_non_contiguous=True)
        xin = [kb.sb("sc_x%d" % i, [128, 12, 516], BF16) for i in range(2)]
        cvb = [kb.sb("sc_cv%d" % i, [128, 512], BF16) for i in range(3)]
        pss = [kb.ps("sc_ps%d" % i, [128, 512]) for i in range(2)]
        pst = [kb.ps("sc_pt%d" % i, [128, 4, 128], BF16) for i in range(2)]
        tok = [kb.sb("sc_tok%d" % i, [128, 4, 1280], BF16) for i in range(2)]
        blk = 0
        cnt = 0
        for (s0, SL, _r) in SEGS:
            N = min(512, SL)
            for t0 in range(0, SL, N):
                x = xin[blk % 2]
                tk = tok[blk % 2]
                blk += 1
                lo = max(t0 - 2, 0)
                hi = min(t0 + N + 2, SL)
                kb.v("memset", x[:], 0.0, writes=[x], eng="gpsimd")
                kb.dma(x[:, :, lo - (t0 - 2):hi - (t0 - 2)],
                       self.XBCT[:, s0 + lo:s0 + hi].rearrange("(c p) t -> p c t", p=128), reads=[self.XBCT], writes=[x])
                for c in range(12):
                    ps = pss[cnt % 2]
                    cv = cvb[cnt % 3]
                    pt = pst[cnt % 2]
                    cnt += 1
                    for k in range(5):
                        kb.mm(ps[:, 0:N], dgs[c][:, k, :], x[:, c, k:k + N], k == 0, k == 4, reads=[dgs[c], x], writes=[ps])
                    kb.act(cv[:, 0:N], ps[:, 0:N], AF.Silu, reads=[ps, cb], writes=[cv], bias=cb[:, c:c + 1])
                    if c >= 8:
                        kb.dma(self.XBCc[(c - 8) * 128:(c - 7) * 128, s0 + t0:s0 + t0 + N], cv[:, 0:N], reads=[cv],
                               writes=[(self.XBCc, (c, s0 + t0))])
                    if c < 10:
                        for tt in range(N // 128):
                            kb.tr(pt[:, tt, :], cv[:, tt * 128:(tt + 1) * 128], self.identb[:], reads=[cv, self.identb],
                                  writes=[(pt, tt)])
                        kb.v("tensor_copy", tk[:, 0:N // 128, c * 128:(c + 1) * 128], pt[:, 0:N // 128, :], reads=[pt],
                             writes=[(tk, c)])
                kb.dma(self.XTOK[s0 + t0:s0 + t0 + N, :].rearrange("(t p) c -> p t c", p=128), tk[:, 0:N // 128, :],
                       reads=[tk], writes=[(self.XTOK, s0 + t0)])
        kb.release(m0)

    def stage_ssm_scan(self, l):
        kb = self.kb
        m0 = kb.mark()
        dtb = kb.sb("ss_dtb", [128, 32], F32)
        kb.dma(dtb[:], self.ssm_dtb[l:l + 1, :].broadcast_to([128, 32]), writes=[dtb])
        nA = kb.sb("ss_nA", [128, 32], F32)
        kb.dma(nA[:], self.ssm_Alog[l:l + 1, :].broadcast_to([128, 32]), writes=[nA])
        kb.act(nA[:], nA[:], AF.Exp, reads=[nA], writes=[nA])
        kb.v("tensor_scalar", nA[:], nA[:], -1.0, None, ALU.mult, reads=[nA], writes=[nA])
        dt = kb.sb("ss_dt", [128, NTILE, 32], F32)
        aa = kb.sb("ss_a", [128, NTILE, 32], F32)
        t1 = kb.sb("ss_t1", [128, NTILE, 32], F32)
        t2 = kb.sb("ss_t2", [128, NTILE, 32], F32)
        kb.dma(dt[:], self.DTt[:, :].rearrange("(t p) c -> p t c", p=128), reads=[self.DTt], writes=[dt])
        bc = lambda tl: tl[:].unsqueeze(1).broadcast_to([128, NTILE, 32])
        kb.v("tensor_tensor", dt[:], dt[:], bc(dtb), ALU.add, reads=[dt, dtb], writes=[dt])
        kb.v("tensor_scalar", t1[:], dt[:], -1.0, None, ALU.mult, reads=[dt], writes=[t1])
        kb.v("tensor_tensor", t1[:], t1[:], dt[:], ALU.max, reads=[t1, dt], writes=[t1])
        kb.act(t1[:], t1[:], AF.Exp, reads=[t1], writes=[t1], scale=-1.0)
        kb.v("tensor_scalar", t1[:], t1[:], 1.0, None, ALU.add, reads=[t1], writes=[t1])
        kb.act(t2[:], t1[:], AF.Ln, reads=[t1], writes=[t2])
        kb.v("tensor_scalar", dt[:], dt[:], 0.0, None, ALU.max, reads=[dt], writes=[dt])
        kb.v("tensor_tensor", dt[:], dt[:], t2[:], ALU.add, reads=[dt, t2], writes=[dt])
        kb.v("tensor_tensor", aa[:], dt[:], bc(nA), ALU.mult, reads=[dt, nA], writes=[aa])

        NS = 4
        xtk = [[kb.sb("ss_x%d_%d" % (s_, i), [128, 1280], BF16) for i in range(2)] for s_ in range(NS)]
        bct = [[kb.sb("ss_bc%d_%d" % (s_, i), [128, 4, 128], BF16) for i in range(2)] for s_ in range(NS)]
        cs_sb = [kb.sb("ss_cs%d" % i, [128, 16], F32) for i in range(NS)]
        dif = [kb.sb("ss_dif%d" % i, [128, 16], F32) for i in range(NS)]
        dIn = [kb.sb("ss_din%d" % i, [128, 16], F32) for i in range(NS)]
        dOut = [kb.sb("ss_dout%d" % i, [128, 16], F32) for i in range(NS)]
        dAs = [kb.sb("ss_dA%d" % i, [128, 16], F32) for i in range(NS)]
        xdt = [kb.sb("ss_xdt%d" % i, [128, 16, 64], BF16) for i in range(NS)]
        xdec = [kb.sb("ss_xdec%d" % i, [128, 16, 64], BF16) for i in range(NS)]
        GTm = [kb.sb("ss_gtm%d" % i, [128, 128], F32) for i in range(4)]
        A1 = [kb.sb("ss_a1%d" % i, [128, 128], F32) for i in range(4)]
        LT = [kb.sb("ss_lt%d" % i, [128, 128], F32) for i in range(4)]
        MT = [kb.sb("ss_mt%d" % i, [128, 128], BF16) for i in range(4)]
        yo = [kb.sb("ss_yo%d" % i, [128, 512], F32) for i in range(4)]
        ych = [kb.sb("ss_ych%d" % i, [128, 1024], F32) for i in range(NS)]
        st32 = [kb.sb("ss_st32%d" % i, [128, 2, 512], F32) for i in range(NS)]
        stbf = [kb.sb("ss_stbf%d" % i, [128, 2, 512], BF16) for i in range(NS)]
        ps_sm = kb.ps("ss_psm0", [128, 32])
        ps_g = kb.ps("ss_pg", [128, 128])
        ps_d = [kb.ps("ss_pd%d" % i, [128, 128]) for i in range(2)]
        ps_y = kb.ps("ss_py", [128, 2, 512])
        ps_off = kb.ps("ss_poff", [128, 512])
        ps_st = kb.ps("ss_pst", [128, 512])
        cnts = {"h": 0, "g": 0}

        def stream(si, b, d):
            tri = self.C(CST_TRIF if d == 0 else CST_TRIB)
            strict = self.C(CST_STRF if d == 0 else CST_STRB)
            kb.v("memset", st32[si][:], 0.0, writes=[st32[si]])
            kb.v("memset", stbf[si][:], 0.0, writes=[stbf[si]])
            ctx_t = [32 + 2 * b, 33 + 2 * b]
            lat_t = [16 * b + c for c in range(16)]
            order = (ctx_t + lat_t) if d == 0 else (ctx_t[::-1] + lat_t[::-1])

            def load(k):
                tt = order[k]
                kb.dma(xtk[si][k % 2][:], self.XTOK[tt * 128:(tt + 1) * 128, :], reads=[self.XTOK], writes=[xtk[si][k % 2]])
                kb.dma(bct[si][k % 2][:], self.XBCc[:, tt * 128:(tt + 1) * 128].rearrange("(c p) t -> p c t", p=128),
                       reads=[self.XBCc], writes=[bct[si][k % 2]])
            load(0)
            for k, tt in enumerate(order):
                if k + 1 < len(order):
                    load(k + 1)
                i = si
                x, bt = xtk[si][k % 2], bct[si][k % 2]
                a_c = aa[:, tt, d * 16:(d + 1) * 16]
                dt_c = dt[:, tt, d * 16:(d + 1) * 16]
                psm = ps_sm
                kb.mm(psm[:, 0:16], tri, a_c, True, True, reads=[aa, self.cstsb], writes=[psm])
                kb.mm(psm[:, 16:32], self.C(CST_ONES), a_c, True, True, reads=[aa, self.cstsb], writes=[psm])
                kb.v("tensor_copy", cs_sb[i][:], psm[:, 0:16], reads=[psm], writes=[cs_sb[i]])
                kb.v("tensor_tensor", dif[i][:], psm[:, 16:32], cs_sb[i][:], ALU.subtract, reads=[psm, cs_sb[i]],
                     writes=[dif[i]])
                kb.act(dAs[i][:], psm[:, 16:32], AF.Exp, reads=[psm], writes=[dAs[i]])
                kb.act(dIn[i][:], cs_sb[i][:], AF.Exp, reads=[cs_sb[i]], writes=[dIn[i]])
                kb.act(dOut[i][:], dif[i][:], AF.Exp, reads=[dif[i]], writes=[dOut[i]])
                xv = x[:, 0:1024].rearrange("p (h e) -> p h e", e=64)
                kb.v("tensor_tensor", xdt[i][:], xv, dt_c.unsqueeze(2).broadcast_to([128, 16, 64]), ALU.mult,
                     reads=[x, dt], writes=[xdt[i]], eng="gpsimd")
                kb.v("tensor_tensor", xdec[i][:], xdt[i][:], dOut[i][:].unsqueeze(2).broadcast_to([128, 16, 64]),
                     ALU.mult, reads=[xdt[i], dOut[i]], writes=[xdec[i]], eng="gpsimd")
                for g in range(2):
                    gm = GTm[cnts["g"] % 4]
                    yg = yo[cnts["g"] % 4]
                    cnts["g"] += 1
                    kb.mm(ps_g[:], bt[:, g, :], bt[:, 2 + g, :], True, True, reads=[bt], writes=[ps_g])
                    kb.v("tensor_tensor", gm[:], ps_g[:], tri, ALU.mult, reads=[ps_g, self.cstsb], writes=[gm])
                    for hh in range(8):
                        h = g * 8 + hh
                        j = cnts["h"] % 4
                        pd = ps_d[cnts["h"] % 2]
                        cnts["h"] += 1
                        kb.v("tensor_scalar", A1[j][:], strict, a_c[:, h:h + 1], None, ALU.mult,
                             reads=[aa, self.cstsb], writes=[A1[j]])
                        kb.mm(pd[:], A1[j][:], tri, True, True, reads=[A1[j], self.cstsb], writes=[pd])
                        kb.act(LT[j][:], pd[:], AF.Exp, reads=[pd], writes=[LT[j]])
                        kb.v("tensor_tensor", MT[j][:], LT[j][:], gm[:], ALU.mult, reads=[LT[j], gm], writes=[MT[j]],
                             eng="gpsimd")
                        kb.mm(ps_y[:, g, hh * 64:(hh + 1) * 64], MT[j][:], xdt[i][:, h, :], True, True,
                              reads=[MT[j], xdt[i]], writes=[(ps_y, g)])
                    kb.mm(ps_off[:], bt[:, 2 + g, :], stbf[si][:, g, :], True, True, reads=[bt, (stbf[si], g)], writes=[ps_off])
                    kb.v("tensor_tensor", yg[:].rearrange("p (h e) -> p h e", e=64),
                         ps_off[:].rearrange("p (h e) -> p h e", e=64),
                         dIn[i][:, g * 8:(g + 1) * 8].unsqueeze(2).broadcast_to([128, 8, 64]), ALU.mult,
                         reads=[ps_off, dIn[i]], writes=[yg])
                    kb.v("tensor_tensor", ych[i][:, g * 512:(g + 1) * 512], ps_y[:, g, :], yg[:], ALU.add,
                         reads=[(ps_y, g), yg], writes=[(ych[i], g)])
                    kb.mm(ps_st[:], x[:, 1024 + g * 128:1024 + (g + 1) * 128],
                          xdec[i][:, g * 8:(g + 1) * 8, :].rearrange("p h e -> p (h e)"), True, True,
                          reads=[x, xdec[i]], writes=[ps_st])
                    kb.v("tensor_tensor", st32[si][:, g, :].rearrange("p (h e) -> p h e", e=64),
                         st32[si][:, g, :].rearrange("p (h e) -> p h e", e=64),
                         dAs[i][:, g * 8:(g + 1) * 8].unsqueeze(2).broadcast_to([128, 8, 64]), ALU.mult,
                         reads=[(st32[si], g), dAs[i]], writes=[(st32[si], g)])
                    kb.v("tensor_tensor", st32[si][:, g, :], st32[si][:, g, :], ps_st[:], ALU.add,
                         reads=[(st32[si], g), ps_st], writes=[(st32[si], g)])
                    kb.act(stbf[si][:, g, :], st32[si][:, g, :], AF.Copy, reads=[(st32[si], g)], writes=[(stbf[si], g)])
                kb.dma(self.YD[d][tt * 128:(tt + 1) * 128, :], ych[i][:], reads=[ych[i]], writes=[(self.YD[d], tt)])
                yield

        gens = [stream(si, si // 2, si % 2) for si in range(NS)]
        alive = list(gens)
        while alive:
            for g_ in list(alive):
                try:
                    next(g_)
                except StopIteration:
                    alive.remove(g_)
        kb.release(m0)

    def stage_ssm_out(self, l):
        kb = self.kb
        m0 = kb.mark()
        Dx = kb.sb("so_D", [128, 1024], F32)
        kb.dma(Dx[:], self.ssm_Dx[l:l + 1, :].broadcast_to([128, 1024]), writes=[Dx])
        ng = kb.sb("so_ng", [128, 1024], F32)
        kb.dma(ng[:], self.ssm_ng[l:l + 1, :].broadcast_to([128, 1024]), writes=[ng])
        yf = [kb.sb("so_yf%d" % i, [128, 1024], F32) for i in range(2)]
        yb = [kb.sb("so_yb%d" % i, [128, 1024], F32) for i in range(2)]
        xx = [kb.sb("so_x%d" % i, [128, 1024], BF16) for i in range(2)]
        zz = [kb.sb("so_z%d" % i, [128, 1024], BF16) for i in range(2)]
        sz = [kb.sb("so_sz%d" % i, [128, 1024], F32) for i in range(2)]
        junk = kb.sb("so_junk", [128, 512], F32)
        ss = [kb.sb("so_ss%d" % i, [128, 2], F32) for i in range(2)]
        yn = [kb.sb("so_yn%d" % i, [128, 1024], BF16) for i in range(2)]
        pt = [kb.ps("so_pt%d" % i, [128, 8, 128], BF16) for i in range(2)]
        yT = [kb.sb("so_yT%d" % i, [128, 8, 128], BF16) for i in range(2)]
        for tt in range(NTILE):
            i = tt % 2
            rows = slice(tt * 128, (tt + 1) * 128)
            kb.dma(yf[i][:], self.YD[0][rows, :], reads=[self.YD[0]], writes=[yf[i]])
            kb.dma(yb[i][:], self.YD[1][rows, :], reads=[self.YD[1]], writes=[yb[i]])
            kb.dma(xx[i][:], self.XTOK[rows, 0:1024], reads=[self.XTOK], writes=[xx[i]])
            kb.dma(zz[i][:], self.Zt[rows, :], reads=[self.Zt], writes=[zz[i]])
            kb.v("tensor_tensor", yf[i][:], yf[i][:], yb[i][:], ALU.add, reads=[yf[i], yb[i]], writes=[yf[i]])
            kb.v("tensor_tensor", yb[i][:], xx[i][:], Dx[:], ALU.mult, reads=[xx[i], Dx], writes=[yb[i]], eng="gpsimd")
            kb.v("tensor_tensor", yf[i][:], yf[i][:], yb[i][:], ALU.add, reads=[yf[i], yb[i]], writes=[yf[i]])
            kb.act(sz[i][:], zz[i][:], AF.Silu, reads=[zz[i]], writes=[sz[i]])
            kb.v("tensor_tensor", yf[i][:], yf[i][:], sz[i][:], ALU.mult, reads=[yf[i], sz[i]], writes=[yf[i]])
            for g in range(2):
                kb.act(junk[:], yf[i][:, g * 512:(g + 1) * 512], AF.Square, reads=[yf[i]], writes=[junk, (ss[i], g)],
                       accum_out=ss[i][:, g:g + 1])
            kb.act(ss[i][:], ss[i][:], AF.Sqrt, reads=[ss[i]], writes=[ss[i]], scale=1.0 / 512, bias=self.epsb[:])
            kb.v("reciprocal", ss[i][:], ss[i][:], reads=[ss[i]], writes=[ss[i]])
            for g in range(2):
                kb.v("scalar_tensor_tensor", yn[i][:, g * 512:(g + 1) * 512], yf[i][:, g * 512:(g + 1) * 512],
                     ss[i][:, g:g + 1], ng[:, g * 512:(g + 1) * 512], ALU.mult, ALU.mult, reads=[yf[i], ss[i], ng],
                     writes=[(yn[i], g)])
            for c in range(8):
                kb.tr(pt[i][:, c, :], yn[i][:, c * 128:(c + 1) * 128], self.identb[:], reads=[yn[i], self.identb],
                      writes=[(pt[i], c)])
            kb.act(yT[i][:], pt[i][:], AF.Copy, reads=[pt[i]], writes=[yT[i]])
            kb.dma(self.YST[:, rows].rearrange("(c p) t -> p c t", p=128), yT[i][:], reads=[yT[i]],
                   writes=[(self.YST, tt)])
        kb.release(m0)

    def stage_conf(self, l):
        kb = self.kb
        m0 = kb.mark()
        dgs = self.build_diags("cf", self.cv_wT[l], 8, 31)
        small = kb.sb("cf_small", [128, 3, 8], F32)
        for j, src in enumerate([self.cv_dw_b, self.cv_ln_g, self.cv_ln_b]):
            kb.dma(small[:, j, :], src[l].rearrange("(c p) -> p c", p=128), writes=[(small, j)], allow_slow_non_contiguous=True)
        ga = [kb.sb("cf_ga%d" % i, [128, 16, 542], BF16) for i in range(2)]
        sig = kb.sb("cf_sig", [128, 8, 542], BF16)
        u = kb.sb("cf_u", [128, 8, 542], BF16)
        hh = kb.sb("cf_hh", [128, 8, 512], F32)
        sq = kb.sb("cf_sq", [128, 8, 512], F32)
        mean = kb.sb("cf_mean", [128, 512], F32)
        m2 = kb.sb("cf_m2", [128, 512], F32)
        rstd = kb.sb("cf_rstd", [128, 512], F32)
        tt1 = [kb.sb("cf_t1%d" % i, [128, 512], F32) for i in range(2)]
        ob = [kb.sb("cf_ob%d" % i, [128, 512], BF16) for i in range(2)]
        pss = [kb.ps("cf_ps%d" % i, [128, 512]) for i in range(2)]
        ps1 = kb.ps("cf_sum1", [128, 512])
        ps2 = kb.ps("cf_sum2", [128, 512])
        blk = 0
        for (s0, SL, _r) in SEGS:
            N = min(512, SL)
            for t0 in range(0, SL, N):
                g = ga[blk % 2]
                blk += 1
                lo = max(t0 - 15, 0)
                hi = min(t0 + N + 15, SL)
                W = N + 30
                kb.v("memset", g[:], 0.0, writes=[g], eng="gpsimd")
                kb.dma(g[:, :, lo - (t0 - 15):hi - (t0 - 15)],
                       self.GLUT[:, s0 + lo:s0 + hi].rearrange("(c p) t -> p c t", p=128), reads=[self.GLUT], writes=[g])
                kb.act(sig[:, :, 0:W], g[:, 8:16, 0:W], AF.Sigmoid, reads=[g], writes=[sig])
                kb.v("tensor_tensor", u[:, :, 0:W], g[:, 0:8, 0:W], sig[:, :, 0:W], ALU.mult, reads=[g, sig], writes=[u])
                for c in range(8):
                    ps = pss[c % 2]
                    for k in range(31):
                        kb.mm(ps[:, 0:N], dgs[c][:, k, :], u[:, c, k:k + N], k == 0, k == 30, reads=[dgs[c], u], writes=[ps])
                    kb.act(hh[:, c, 0:N], ps[:, 0:N], AF.Identity, reads=[ps, small], writes=[(hh, c)], bias=small[:, 0, c:c + 1])
                    kb.act(sq[:, c, 0:N], hh[:, c, 0:N], AF.Square, reads=[(hh, c)], writes=[(sq, c)])
                for c in range(8):
                    kb.mm(ps1[:, 0:N], self.C(CST_ONES), hh[:, c, 0:N], c == 0, c == 7, reads=[(hh, c), self.cstsb], writes=[ps1])
                for c in range(8):
                    kb.mm(ps2[:, 0:N], self.C(CST_ONES), sq[:, c, 0:N], c == 0, c == 7, reads=[(sq, c), self.cstsb], writes=[ps2])
                kb.v("tensor_scalar", mean[:, 0:N], ps1[:, 0:N], 1.0 / 1024, None, ALU.mult, reads=[ps1], writes=[mean])
                kb.v("tensor_tensor", m2[:, 0:N], mean[:, 0:N], mean[:, 0:N], ALU.mult, reads=[mean], writes=[m2])
                kb.v("scalar_tensor_tensor", rstd[:, 0:N], ps2[:, 0:N], 1.0 / 1024, m2[:, 0:N], ALU.mult, ALU.subtract,
                     reads=[ps2, m2], writes=[rstd])
                kb.act(rstd[:, 0:N], rstd[:, 0:N], AF.Sqrt, reads=[rstd], writes=[rstd], bias=self.epsb[:])
                kb.v("reciprocal", rstd[:, 0:N], rstd[:, 0:N], reads=[rstd], writes=[rstd])
                for c in range(8):
                    t1 = tt1[c % 2]
                    o = ob[c % 2]
                    kb.v("tensor_tensor", t1[:, 0:N], hh[:, c, 0:N], mean[:, 0:N], ALU.subtract, reads=[(hh, c), mean],
                         writes=[t1], eng="gpsimd")
                    kb.v("tensor_tensor", t1[:, 0:N], t1[:, 0:N], rstd[:, 0:N], ALU.mult, reads=[t1, rstd], writes=[t1])
                    kb.act(o[:, 0:N], t1[:, 0:N], AF.Silu, reads=[t1, small], writes=[o], scale=small[:, 1, c:c + 1],
                           bias=small[:, 2, c:c + 1])
                    kb.dma(self.CVAT[c * 128:(c + 1) * 128, s0 + t0:s0 + t0 + N], o[:, 0:N], reads=[o],
                           writes=[(self.CVAT, (c, s0 + t0))])
        kb.release(m0)

    def stage_merge(self, l, xin, xout):
        kb = self.kb
        m0 = kb.mark()
        g1 = []
        for r in range(3):
            t = kb.sb("mg_g1%d" % r, [128, D], F32)
            kb.dma(t[:], self.modd[r:r + 1, 2 * D:3 * D].broadcast_to([128, D]), reads=[self.modd], writes=[t])
            g1.append(t)
        bo = kb.sb("mg_bo", [128, 16], F32)
        kb.dma(bo[:], self.cv_bo[l].rearrange("(c p) -> p c", p=128), writes=[bo], allow_slow_non_contiguous=True)
        aot = [kb.sb("mg_ao%d" % i, [128, 1024], BF16) for i in range(2)]
        AOT = kb.sb("mg_AOT", [128, 8, 512], BF16)
        YS = kb.sb("mg_YS", [128, 8, 512], BF16)
        CV = kb.sb("mg_CV", [128, 8, 512], BF16)
        wts = [kb.sb("mg_w%d" % i, [128, 3, 8, 128], BF16) for i in range(2)]
        gts = [kb.sb("mg_gt%d" % i, [128, 3, 512], BF16) for i in range(2)]
        sg = [kb.sb("mg_sg%d" % i, [128, 3, 512], F32) for i in range(2)]
        ma = [kb.sb("mg_ma%d" % i, [128, 512], F32) for i in range(2)]
        mb = [kb.sb("mg_mb%d" % i, [128, 512], F32) for i in range(2)]
        mc = [kb.sb("mg_mc%d" % i, [128, 512], F32) for i in range(2)]
        MT = kb.sb("mg_MT", [128, 16, 512], BF16)
        wo = [kb.sb("mg_wo%d" % i, [128, 16, 512], BF16) for i in range(2)]
        xt = kb.sb("mg_xt", [128, 4, D], F32)
        tmp = [kb.sb("mg_tmp%d" % i, [128, 512], F32) for i in range(2)]
        pt = [kb.ps("mg_pt%d" % i, [128, 8, 128], BF16) for i in range(2)]
        psa = kb.ps("mg_psa", [128, 512])
        pss_ = kb.ps("mg_pss", [128, 512])
        psc = kb.ps("mg_psc", [128, 512])
        pso = [kb.ps("mg_pso%d" % i, [128, 512]) for i in range(2)]
        cnt = 0
        for tg in range(9):
            t0 = tg * 512
            r = 0 if tg < 4 else (1 if tg < 8 else 2)
            for tt in range(4):
                a = aot[tt % 2]
                p = pt[tt % 2]
                kb.dma(a[:], self.AO[t0 + tt * 128:t0 + (tt + 1) * 128, :], reads=[self.AO], writes=[a])
                for c in range(8):
                    kb.tr(p[:, c, :], a[:, c * 128:(c + 1) * 128], self.identb[:], reads=[a, self.identb], writes=[(p, c)])
                kb.act(AOT[:, :, tt * 128:(tt + 1) * 128], p[:], AF.Copy, reads=[p], writes=[(AOT, tt)])
            kb.dma(YS[:], self.YST[:, t0:t0 + 512].rearrange("(c p) t -> p c t", p=128), reads=[self.YST], writes=[YS])
            kb.dma(CV[:], self.CVAT[:, t0:t0 + 512].rearrange("(c p) t -> p c t", p=128), reads=[self.CVAT], writes=[CV])
            kb.dma(xt[:], xin[t0:t0 + 512, :].rearrange("(t p) d -> p t d", p=128), reads=[xin], writes=[xt])
            for fc in range(16):
                i = fc % 2
                w = wts[i]
                for j, src in enumerate([self.NAW[l], self.SSW[l], self.CVW[l]]):
                    kb.dma(w[:, j, :, :], src[:, fc * 128:(fc + 1) * 128].rearrange("(c p) f -> p c f", p=128),
                           reads=[src], writes=[(w, j)])
                kb.dma(gts[i][:], self.GATET[:, t0:t0 + 512].rearrange("(j c p) t -> p j c t", j=3, p=128)[:, :, fc, :],
                       reads=[self.GATET], writes=[gts[i]])
                kb.act(sg[i][:], gts[i][:], AF.Sigmoid, reads=[gts[i]], writes=[sg[i]])
                for j, (ps, src) in enumerate([(psa, AOT), (pss_, YS), (psc, CV)]):
                    for c in range(8):
                        kb.mm(ps[:], w[:, j, c, :], src[:, c, :], c == 0, c == 7, reads=[w, src], writes=[ps])
                kb.v("tensor_tensor", ma[i][:], psa[:], sg[i][:, 0, :], ALU.mult, reads=[psa, sg[i]], writes=[ma[i]])
                kb.v("tensor_tensor", mb[i][:], pss_[:], sg[i][:, 1, :], ALU.mult, reads=[pss_, sg[i]], writes=[mb[i]])
                kb.v("scalar_tensor_tensor", mc[i][:], psc[:], bo[:, fc:fc + 1], sg[i][:, 2, :], ALU.add, ALU.mult,
                     reads=[psc, bo, sg[i]], writes=[mc[i]])
                kb.v("tensor_tensor", ma[i][:], ma[i][:], mb[i][:], ALU.add, reads=[ma[i], mb[i]], writes=[ma[i]], eng="gpsimd")
                kb.v("tensor_tensor", MT[:, fc, :], ma[i][:], mc[i][:], ALU.add, reads=[ma[i], mc[i]], writes=[(MT, fc)],
                     eng="gpsimd")
            for nb in range(4):
                wb = wo[nb % 2]
                kb.dma(wb[:], self.WO[l][:, nb * 512:(nb + 1) * 512].rearrange("(c p) n -> p c n", p=128),
                       reads=[self.WO[l]], writes=[wb])
                for tt in range(4):
                    po = pso[cnt % 2]
                    tm = tmp[cnt % 2]
                    cnt += 1
                    for fc in range(16):
                        kb.mm(po[:], MT[:, fc, tt * 128:(tt + 1) * 128], wb[:, fc, :], fc == 0, fc == 15,
                              reads=[MT, wb], writes=[po])
                    cs = slice(nb * 512, (nb + 1) * 512)
                    kb.v("tensor_tensor", tm[:], po[:], g1[r][:, cs], ALU.mult, reads=[po, g1[r]], writes=[tm])
                    kb.v("tensor_tensor", xt[:, tt, cs], xt[:, tt, cs], tm[:], ALU.add, reads=[(xt, (tt, nb)), tm],
                         writes=[(xt, (tt, nb))], eng="gpsimd")
            kb.dma(xout[t0:t0 + 512, :].rearrange("(t p) d -> p t d", p=128), xt[:], reads=[xt], writes=[(xout, tg)])
        kb.release(m0)

    def top16_multi(self, items):
        kb = self.kb
        for (va, wa, vo, io, rd, wr) in items:
            kb.v("max", vo[:, 0:8], va, reads=rd, writes=wr)
        for (va, wa, vo, io, rd, wr) in items:
            kb.v("max_index", io[:, 0:8], vo[:, 0:8], va, reads=rd + wr, writes=wr)
        for (va, wa, vo, io, rd, wr) in items:
            kb.v("match_replace", wa, vo[:, 0:8], va, -1e30, reads=rd + wr, writes=wr)
        for (va, wa, vo, io, rd, wr) in items:
            kb.v("max", vo[:, 8:16], wa, reads=wr, writes=wr)
        for (va, wa, vo, io, rd, wr) in items:
            kb.v("max_index", io[:, 8:16], vo[:, 8:16], wa, reads=wr, writes=wr)

    def stage_peer_route(self, l):
        kb = self.kb
        m0 = kb.mark()
        kT = kb.sb("pr_kT", [128, 16, 128], F32)
        kb.dma(kT[:], self.keysT[l], writes=[kT])
        io16 = self.C(CST_IOTA)[:, 0:16]
        hb = [kb.sb("pr_h%d" % i, [128, 16, 512], BF16) for i in range(1)]
        wq = [kb.sb("pr_wq%d" % i, [128, 16, 128], BF16) for i in range(2)]
        qT = kb.sb("pr_qT", [128, 16, 512], F32)
        sc = kb.sb("pr_sc", [128, 16, 128], F32)
        scw = kb.sb("pr_scw", [128, 16, 128], F32)
        stop = kb.sb("pr_stop", [128, 16, 16], F32)
        itop = kb.sb("pr_itop", [128, 16, 16], U32)
        itf = kb.sb("pr_itf", [128, 16, 16], F32)
        cand = kb.sb("pr_cand", [128, 8, 256], F32)
        candw = kb.sb("pr_candw", [128, 8, 256], F32)
        best = kb.sb("pr_best", [128, 8, 16], F32)
        pos = kb.sb("pr_pos", [128, 8, 16], U32)
        pu = kb.sb("pr_pu", [128, 2, 8, 16], U32)
        pf = kb.sb("pr_pf", [128, 2, 8, 16], F32)
        eq = [kb.sb("pr_eq0", [128, 8, 16, 16], F32)] * 2
        sel = kb.sb("pr_sel", [128, 3, 128], F32)
        ex = kb.sb("pr_ex", [128, 8, 16], F32)
        sm = kb.sb("pr_sm", [128, 8], F32)
        selT = [kb.sb("pr_selT%d" % i, [128, 3, 128], F32) for i in range(2)]
        TQ = 16
        Abig = [kb.sb("pr_A%d" % i, [128, TQ, 128], BF16) for i in range(2)]
        Bbig = [kb.sb("pr_B%d" % i, [128, TQ, 128], BF16) for i in range(2)]
        WDsb = [kb.sb("pr_WD0", [128, 128, 256], BF16)] * 2
        ps_q = kb.ps("pr_psq", [128, 512])
        ps_s = kb.ps("pr_pss", [128, 16, 128])
        ps_t = kb.ps("pr_pst", [128, 3, 128])
        ps_w = [kb.ps("pr_psw%d" % i, [128, 4, 128]) for i in range(2)]
        iota3 = self.C(CST_IOTA).unsqueeze(1).broadcast_to([128, TQ, 128])
        cn = {'ab': 0, 'wi': 0}
        def front(tg, tt):
            if True:
                tile_i = tg * 4 + tt
                sT = selT[tile_i % 2]
                for fc in range(16):
                    kb.mm(ps_s[:, fc, :], qT[:, fc, tt * 128:(tt + 1) * 128], kT[:, fc, :], True, True,
                          reads=[(qT, fc), kT], writes=[ps_s])
                kb.act(sc[:], ps_s[:], AF.Copy, reads=[ps_s], writes=[sc])
                self.top16_multi([(sc[:, fc, :], scw[:, fc, :], stop[:, fc, :], itop[:, fc, :], [sc],
                                   [(scw, fc), (stop, fc), (itop, fc)]) for fc in range(16)])
                kb.v("tensor_copy", itf[:], itop[:], reads=[itop], writes=[itf])
                sv = stop[:].rearrange("p (h j) i -> p h j i", j=2)
                iv = itf[:].rearrange("p (h j) i -> p h j i", j=2)
                kb.v("tensor_tensor", cand[:].rearrange("p h (i j) -> p h i j", j=16),
                     sv[:, :, 0, :].unsqueeze(3).broadcast_to([128, 8, 16, 16]),
                     sv[:, :, 1, :].unsqueeze(2).broadcast_to([128, 8, 16, 16]), ALU.add, reads=[stop], writes=[cand])
                self.top16_multi([(cand[:, hh, :], candw[:, hh, :], best[:, hh, :], pos[:, hh, :], [cand],
                                   [(candw, hh), (best, hh), (pos, hh)]) for hh in range(8)])
                kb.v("tensor_single_scalar", pu[:, 0], pos[:], 4, ALU.logical_shift_right, reads=[pos], writes=[(pu, 0)])
                kb.v("tensor_single_scalar", pu[:, 1], pos[:], 15, ALU.bitwise_and, reads=[pos], writes=[(pu, 1)])
                kb.v("tensor_copy", pf[:], pu[:], reads=[pu], writes=[pf])
                for j in range(2):
                    e = eq[j]
                    kb.v("tensor_tensor", e[:], io16.unsqueeze(1).unsqueeze(1).broadcast_to([128, 8, 16, 16]),
                         pf[:, j].unsqueeze(3).broadcast_to([128, 8, 16, 16]), ALU.is_equal, reads=[pf, self.cstsb], writes=[e])
                    kb.v("tensor_tensor", e[:], e[:], iv[:, :, j, :].unsqueeze(2).broadcast_to([128, 8, 16, 16]), ALU.mult,
                         reads=[e, itf], writes=[e])
                    kb.v("tensor_reduce", sel[:, j, :].rearrange("p (h m) -> p h m", m=16), e[:], AX.X, ALU.add,
                         reads=[e], writes=[(sel, j)])
                kb.v("tensor_tensor", ex[:], best[:], best[:, :, 0:1].broadcast_to([128, 8, 16]), ALU.subtract,
                     reads=[best], writes=[ex])
                kb.act(ex[:], ex[:], AF.Exp, reads=[ex], writes=[ex])
                kb.v("tensor_reduce", sm[:], ex[:], AX.X, ALU.add, reads=[ex], writes=[sm])
                kb.v("reciprocal", sm[:], sm[:], reads=[sm], writes=[sm])
                kb.v("tensor_tensor", sel[:, 2, :].rearrange("p (h m) -> p h m", m=16), ex[:],
                     sm[:].unsqueeze(2).broadcast_to([128, 8, 16]), ALU.mult, reads=[ex, sm], writes=[(sel, 2)])
                if "SEL" in self.debug:
                    kb.dma(self.SEL[tile_i * 128:(tile_i + 1) * 128], sel[:], reads=[sel], writes=[(self.SEL, tile_i)])
                for j in range(3):
                    kb.tr(ps_t[:, j, :], sel[:, j, :], self.C(CST_IDENT), reads=[sel, self.cstsb], writes=[(ps_t, j)])
                kb.act(sT[:], ps_t[:], AF.Copy, reads=[ps_t], writes=[sT])

        def back(tile_i):
            if True:
                sT = selT[tile_i % 2]
                WD = WDsb[tile_i % 2]
                for qq in range(128 // TQ):
                    A, B = Abig[cn['ab'] % 2], Bbig[cn['ab'] % 2]
                    cn['ab'] += 1
                    ts_ = slice(qq * TQ, (qq + 1) * TQ)
                    kb.v("tensor_tensor", A[:], iota3, sT[:, 0, ts_].unsqueeze(2).broadcast_to([128, TQ, 128]), ALU.is_equal,
                         reads=[sT, self.cstsb], writes=[A])
                    kb.v("tensor_tensor", A[:], A[:], sT[:, 2, ts_].unsqueeze(2).broadcast_to([128, TQ, 128]), ALU.mult,
                         reads=[A, sT], writes=[A], eng="gpsimd")
                    kb.v("tensor_tensor", B[:], iota3, sT[:, 1, ts_].unsqueeze(2).broadcast_to([128, TQ, 128]), ALU.is_equal,
                         reads=[sT, self.cstsb], writes=[B])
                    for q4 in range(TQ // 4):
                        pw = ps_w[cn['wi'] % 2]
                        cn['wi'] += 1
                        for tq in range(4):
                            t = q4 * 4 + tq
                            kb.mm(pw[:, tq, :], B[:, t, :], A[:, t, :], True, True, reads=[A, B], writes=[pw])
                        tb = (tile_i % 2) * 128 + qq * TQ + q4 * 4
                        kb.act(WD[:, :, tb:tb + 4], pw[:].rearrange("p t k -> p k t"), AF.Copy, reads=[pw],
                               writes=[(WD, tb)])
                if tile_i % 2 == 1:
                    pr = tile_i // 2
                    kb.dma(self.WDs[:, :, pr * 256:(pr + 1) * 256].rearrange("a b t -> b a t"), WD[:], reads=[WD],
                           writes=[(self.WDs, pr)])

        for tg in range(9):
            h = hb[0]
            t0 = tg * 512
            kb.dma(h[:], self.HT[:, t0:t0 + 512].rearrange("(c p) t -> p c t", p=128), reads=[self.HT], writes=[h])
            for fc in range(16):
                w = wq[fc % 2]
                kb.dma(w[:], self.WQ[l][:, fc * 128:(fc + 1) * 128].rearrange("(c p) f -> p c f", p=128),
                       reads=[self.WQ[l]], writes=[w])
                for c in range(16):
                    kb.mm(ps_q[:], w[:, c, :], h[:, c, :], c == 0, c == 15, reads=[w, h], writes=[ps_q])
                kb.act(qT[:, fc, :], ps_q[:], AF.Copy, reads=[ps_q], writes=[(qT, fc)])
            for tt in range(4):
                tile_i = tg * 4 + tt
                front(tg, tt)
                if tile_i >= 1:
                    back(tile_i - 1)
        back(NTILE - 1)
        kb.release(m0)

    def stage_peer_dense(self, l, xin, xout):
        kb = self.kb
        m0 = kb.mark()
        TP = 768
        NSUB = 2
        SUB = TP // NSUB
        NTT = TP // 128
        g2 = [kb.sb("pd_g2%d" % r, [128, D], F32) for r in range(1)]
        hp = kb.sb("pd_h", [128, 16, TP], BF16)
        acc = kb.sb("pd_acc", [128, NTT, D], F32)
        ut = [kb.sb("pd_ut%d" % i, [128, 16, 512], BF16) for i in range(2)]
        vg = [kb.sb("pd_vg%d" % i, [128, 4, D], BF16) for i in range(2)]
        wd = [kb.sb("pd_wd%d" % i, [128, 4, TP], BF16) for i in range(2)]
        GT = [kb.sb("pd_GT%d" % i, [128, 4, TP], BF16) for i in range(2)]
        ge = [kb.sb("pd_ge%d" % i, [128, SUB], BF16) for i in range(4)]
        xt = [kb.sb("pd_x%d" % i, [128, D], F32) for i in range(2)]
        ps_u = [kb.ps("pd_pu%d" % i, [128, SUB]) for i in range(4)]
        ps_v = [kb.ps("pd_pv%d" % i, [128, 512]) for i in range(4)]
        gi = 0
        vi = 0
        NP = NT // TP
        work = [(p, eg) for p in range(NP) for eg in range(32)]

        def load(i):
            p, eg = work[i]
            j = i % 2
            e0 = eg * 512
            t0 = p * TP
            kb.dma(ut[j][:], self.UTB[l][:, e0:e0 + 512].rearrange("(c p) e -> p c e", p=128), reads=[self.UTB[l]],
                   writes=[ut[j]])
            kb.dma(vg[j][:], self.VB[l][e0:e0 + 512, :].rearrange("(a b) d -> b a d", b=128), reads=[self.VB[l]],
                   writes=[vg[j]])
            kb.dma(wd[j][:], self.WDs[eg * 4:(eg + 1) * 4, :, t0:t0 + TP].rearrange("a b t -> b a t"),
                   reads=[self.WDs], writes=[wd[j]])
        load(0)
        for i, (p, eg) in enumerate(work):
            t0 = p * TP
            j = i % 2
            if eg == 0:
                kb.dma(hp[:], self.HT[:, t0:t0 + TP].rearrange("(c p) t -> p c t", p=128), reads=[self.HT], writes=[hp])
                kb.v("memset", acc[:], 0.0, writes=[acc], eng="gpsimd")
            if i + 1 < len(work):
                load(i + 1)
            G = GT[j]
            for k1 in range(4):
                for sb_ in range(NSUB):
                    pu = ps_u[gi % 4]
                    g = ge[gi % 4]
                    gi += 1
                    ts_ = slice(sb_ * SUB, (sb_ + 1) * SUB)
                    for c in range(16):
                        kb.mm(pu[:], ut[j][:, c, k1 * 128:(k1 + 1) * 128], hp[:, c, ts_], c == 0, c == 15,
                              reads=[ut[j], hp], writes=[pu])
                    kb.act(g[:], pu[:], AF.Gelu, reads=[pu], writes=[g])
                    kb.v("tensor_tensor", G[:, k1, ts_], g[:], wd[j][:, k1, ts_], ALU.mult, reads=[g, wd[j]],
                         writes=[(G, (k1, sb_))], eng="gpsimd")
            for tt in range(NTT):
                for nb in range(4):
                    pv = ps_v[vi % 4]
                    vi += 1
                    for k1 in range(4):
                        kb.mm(pv[:], G[:, k1, tt * 128:(tt + 1) * 128], vg[j][:, k1, nb * 512:(nb + 1) * 512],
                              k1 == 0, k1 == 3, reads=[G, vg[j]], writes=[pv])
                    cs = slice(nb * 512, (nb + 1) * 512)
                    kb.v("tensor_tensor", acc[:, tt, cs], acc[:, tt, cs], pv[:], ALU.add, reads=[(acc, (tt, nb)), pv],
                         writes=[(acc, (tt, nb))])
            if eg == 31:
                for tt in range(NTT):
                    x = xt[tt % 2]
                    tok0 = t0 + tt * 128
                    r = 0 if tok0 < 2048 else (1 if tok0 < 4096 else 2)
                    rows = slice(tok0, tok0 + 128)
                    if tt == 0 or tok0 in (2048, 4096):
                        kb.dma(g2[0][:], self.modd[r:r + 1, 5 * D:6 * D].broadcast_to([128, D]), reads=[self.modd],
                               writes=[g2[0]])
                    kb.dma(x[:], xin[rows, :], reads=[xin], writes=[x])
                    kb.v("tensor_tensor", acc[:, tt, :], acc[:, tt, :], g2[0][:], ALU.mult, reads=[acc, g2[0]], writes=[acc])
                    kb.v("tensor_tensor", x[:], x[:], acc[:, tt, :], ALU.add, reads=[x, acc], writes=[x])
                    kb.dma(xout[rows, :], x[:], reads=[x], writes=[(xout, (p, tt))])
        kb.release(m0)

    def build(self):
        kb = self.kb
        self.declare()
        self.load_consts()
        self.epsb = kb.sb("epsb", [128, 1], F32)
        kb.v("memset", self.epsb[:], EPS, writes=[self.epsb])
        kb.cur_stage = "prologue"
        kb.profile_scopes = self.profile
        self.prologue()
        st = self.stages
        xcur = self.x_in

        def S(name):
            kb.cur_stage = name
        for l in range(self.nlayers):
            if st is None or "mod" in st:
                S("mod%d" % l)
                self.stage_mod(l)
            if st is None or "norm1" in st:
                S("norm1_%d" % l)
                self.stage_norm(xcur, self.norm1_g[l:l + 1, :], 0, 1, self.HT)
            if st is None or "proj" in st:
                S("proj%d" % l)
                self.layer_casts(l)
                self.stage_proj(l)
            if st is None or "attn" in st:
                S("attn%d" % l)
                self.stage_attn(l)
            if st is None or "ssm" in st:
                S("ssmconv%d" % l)
                self.stage_ssm_conv(l)
                S("ssmscan%d" % l)
                self.stage_ssm_scan(l)
                S("ssmout%d" % l)
                self.stage_ssm_out(l)
            if st is None or "conf" in st:
                S("conf%d" % l)
                self.stage_conf(l)
            if st is None or "merge" in st:
                S("merge%d" % l)
                self.stage_merge(l, xcur, self.xs[0])
            if st is None or "peer" in st:
                S("norm2_%d" % l)
                self.stage_norm(self.xs[0], self.norm2_g[l:l + 1, :], 3, 4, self.HT)
                S("route%d" % l)
                self.stage_peer_route(l)
                S("dense%d" % l)
                self.stage_peer_dense(l, self.xs[0], self.xs[1])
                xcur = self.xs[1]
        S("final")
        if st is None or "final" in st:
            self.stage_norm(xcur, self.final_g[0:1, :], 0, 0, None, final_out=self.out)
        kb.barrier()
        return kb.build(final_waits=self.finals)


def host_inputs(inputs, core):
    b0, b1 = 2 * core, 2 * core + 1
    x = inputs["x"]
    ctx = inputs["ctx"]
    m = {}
    m["x_c"] = np.ascontiguousarray(np.concatenate([x[b0], x[b1], ctx[b0], ctx[b1]], axis=0))
    m["c3T"] = np.ascontiguousarray(np.stack([inputs["c"][b0], inputs["c"][b1], inputs["c_ctx"]], axis=1))
    return m


def shared_inputs(inputs):
    m = {}
    m["cst"] = host_consts()
    for k in ("w_mod", "b_mod", "norm1_g", "norm2_g", "w_in"):
        m[k] = np.ascontiguousarray(inputs[k])
    m["final_norm_g"] = np.ascontiguousarray(inputs["final_norm_g"].reshape(1, D))
    m["biasT"] = host_bias_tables(inputs["na_rpb"])
    m["ssm_cwT"] = np.ascontiguousarray(inputs["ssm_conv_w"].transpose(0, 2, 1))
    m["ssm_conv_b"] = np.ascontiguousarray(inputs["ssm_conv_b"])
    m["ssm_dtb"] = np.ascontiguousarray(inputs["ssm_dt_bias"].reshape(2, 32))
    m["ssm_Alog"] = np.ascontiguousarray(inputs["ssm_A_log"].reshape(2, 32))
    m["ssm_Dx"] = np.ascontiguousarray(np.repeat(inputs["ssm_D"], 64, axis=1))
    m["ssm_norm_g"] = np.ascontiguousarray(inputs["ssm_norm_g"])
    m["cv_wT"] = np.ascontiguousarray(inputs["cv_dw_w"].transpose(0, 2, 1))
    m["keysT"] = np.ascontiguousarray(inputs["peer_keys"].reshape(2, 16, 128, 128).transpose(0, 3, 1, 2))
    m["peer_uT"] = np.ascontiguousarray(inputs["peer_u"].transpose(0, 2, 1))
    for k in ("cv_dw_b", "cv_ln_g", "cv_ln_b", "cv_bo", "na_wo", "ssm_wo", "cv_wo", "w_out", "peer_wq", "peer_v"):
        m[k] = np.ascontiguousarray(inputs[k])
    return m


def run(inputs, cores=8, debug=(), nlayers=2, stages=None, trace=False, profile=False):
    p = Prog(debug=debug, nlayers=nlayers, stages=stages, profile=profile)
    nc = p.build()
    sh = shared_inputs(inputs)
    in_maps = []
    for c in range(cores):
        m = dict(sh)
        m.update(host_inputs(inputs, c))
        in_maps.append(m)
    res = run_bass_kernel_spmd(nc, in_maps, core_ids=list(range(cores)), trace=trace)
    return res


def kernel(**inputs):
    inputs = {k: np.asarray(v) for k, v in inputs.items()}
    res = run(inputs)
    out = np.empty((16, 2048, D), np.float32)
    for c in range(8):
        o = res.results[c]["out"]
        out[2 * c] = o[:2048]
        out[2 * c + 1] = o[2048:]
    return out
```
